# Optimizing a Trainium2 kernel written in Bass

```python
import jax
import jax.numpy as jnp
from jax import lax
import numpy as np

D_MODEL = 2048
BATCH = 4
SEQ = 4096
DEPTH = 1

ATTN_HEADS = 8
HEAD_DIM = 128
ATTN_WIDTH = ATTN_HEADS * HEAD_DIM
CONV_WIDTH = D_MODEL - ATTN_WIDTH
CONV_GROUPS = 8
CONV_KERNEL = 31
Q_BLOCK = 128
IN_COLS = 3 * ATTN_WIDTH + ATTN_HEADS + 2 * CONV_WIDTH
N_EXPERTS = 32
TOP_K = 4
D_FF = D_MODEL
SWIGLU_LIMIT = 7.0
SWIGLU_ALPHA = 1.702
EXPERT_BLOCK = 128
EPS = 1e-6

kernel_name = "hybrid_fox_conformer_moe_block"


def rms_norm(x, g):
    xf = x.astype(jnp.float32)
    y = xf * lax.rsqrt(jnp.mean(xf * xf, axis=-1, keepdims=True) + EPS)
    return (y * g.astype(jnp.float32)).astype(x.dtype)


def layer_norm(x, g, b):
    xf = x.astype(jnp.float32)
    mu = jnp.mean(xf, axis=-1, keepdims=True)
    var = jnp.mean(jnp.square(xf - mu), axis=-1, keepdims=True)
    y = (xf - mu) * lax.rsqrt(var + EPS)
    return (y * g.astype(jnp.float32) + b.astype(jnp.float32)).astype(x.dtype)


def fox_attention(q, k, v, cum):
    b, h, s, dh = q.shape
    nb = s // Q_BLOCK
    scale = dh ** -0.5
    q_blocks = q.reshape(b, h, nb, Q_BLOCK, dh).transpose(2, 0, 1, 3, 4)
    c_blocks = cum.reshape(b, h, nb, Q_BLOCK).transpose(2, 0, 1, 3)
    key_pos = jnp.arange(s)

    def one_block(args):
        i, q_i, c_i = args
        logits = jnp.einsum("bhqd,bhkd->bhqk", q_i, k).astype(jnp.float32) * scale
        logits = logits + (c_i[..., :, None] - cum[..., None, :])
        q_pos = i * Q_BLOCK + jnp.arange(Q_BLOCK)
        logits = jnp.where(key_pos[None, :] <= q_pos[:, None], logits, -jnp.inf)
        p = jax.nn.softmax(logits, axis=-1).astype(v.dtype)
        return jnp.einsum("bhqk,bhkd->bhqd", p, v)

    out = lax.map(one_block, (jnp.arange(nb), q_blocks, c_blocks))
    return out.transpose(1, 0, 3, 2, 4).reshape(b, s, h * dh)


def hybrid_mixer(h, w_in, b_f, q_norm_g, k_norm_g, conv_w, conv_b, conv_ln_g, conv_ln_b, w_out):
    b, s, _ = h.shape
    proj = h @ w_in
    q, k, v, f_logit, conv_in = jnp.split(
        proj, [ATTN_WIDTH, 2 * ATTN_WIDTH, 3 * ATTN_WIDTH, 3 * ATTN_WIDTH + ATTN_HEADS], axis=-1)

    q = rms_norm(q.reshape(b, s, ATTN_HEADS, HEAD_DIM), q_norm_g).transpose(0, 2, 1, 3)
    k = rms_norm(k.reshape(b, s, ATTN_HEADS, HEAD_DIM), k_norm_g).transpose(0, 2, 1, 3)
    v = v.reshape(b, s, ATTN_HEADS, HEAD_DIM).transpose(0, 2, 1, 3)
    log_f = jax.nn.log_sigmoid((f_logit + b_f).astype(jnp.float32))
    cum = jnp.cumsum(log_f, axis=1).transpose(0, 2, 1)
    attn_out = fox_attention(q, k, v, cum)

    a, g = jnp.split(conv_in, 2, axis=-1)
    u = a * jax.nn.sigmoid(g)
    u = lax.conv_general_dilated(
        u, conv_w[:, None, :], window_strides=(1,), padding=[(CONV_KERNEL - 1, 0)],
        dimension_numbers=("NWC", "WIO", "NWC"), feature_group_count=CONV_WIDTH) + conv_b
    conv_out = jax.nn.silu(layer_norm(u, conv_ln_g, conv_ln_b))

    return jnp.concatenate([attn_out, conv_out], axis=-1) @ w_out


def moe_ffn(h, w_router, b_router, w_gate, b_gate, w_up, b_up, w_down, b_down):
    b, s, d = h.shape
    xf = h.reshape(b * s, d)
    n_tok = b * s
    n_assign = n_tok * TOP_K
    n_blocks = n_assign // EXPERT_BLOCK + N_EXPERTS
    logits = (xf @ w_router + b_router).astype(jnp.float32)
    top_val, top_idx = lax.top_k(logits, TOP_K)
    top_w = jax.nn.softmax(top_val, axis=-1).astype(h.dtype)

    flat_e = top_idx.reshape(n_assign).astype(jnp.int32)
    flat_tok = jnp.arange(n_assign, dtype=jnp.int32) // TOP_K
    flat_w = top_w.reshape(n_assign)
    order = jnp.argsort(flat_e, stable=True)
    sorted_e = flat_e[order]
    sizes = jnp.bincount(flat_e, length=N_EXPERTS).astype(jnp.int32)
    start = jnp.cumsum(sizes) - sizes
    padded = (sizes + EXPERT_BLOCK - 1) // EXPERT_BLOCK * EXPERT_BLOCK
    padded_end = jnp.cumsum(padded)
    padded_start = padded_end - padded
    dest = padded_start[sorted_e] + jnp.arange(n_assign, dtype=jnp.int32) - start[sorted_e]
    n_slots = n_blocks * EXPERT_BLOCK
    slot_tok = jnp.zeros((n_slots,), jnp.int32).at[dest].set(flat_tok[order])
    slot_w = jnp.zeros((n_slots,), h.dtype).at[dest].set(flat_w[order])
    block_e = jnp.minimum(
        jnp.searchsorted(padded_end, jnp.arange(n_blocks, dtype=jnp.int32) * EXPERT_BLOCK, side="right"),
        N_EXPERTS - 1).astype(jnp.int32)

    def expert_block(y, args):
        tok, wt, e = args
        xb = xf[tok]
        g = jnp.minimum(xb @ w_gate[e] + b_gate[e], SWIGLU_LIMIT)
        u = jnp.clip(xb @ w_up[e] + b_up[e], -SWIGLU_LIMIT, SWIGLU_LIMIT)
        hid = (u + 1) * (g * jax.nn.sigmoid(SWIGLU_ALPHA * g))
        out = hid @ w_down[e] + b_down[e]
        return y.at[tok].add(out * wt[:, None]), None

    y, _ = lax.scan(
        expert_block, jnp.zeros_like(xf),
        (slot_tok.reshape(n_blocks, EXPERT_BLOCK), slot_w.reshape(n_blocks, EXPERT_BLOCK), block_e))
    return y.reshape(b, s, d)


def setup_inputs(seed: int = 0) -> dict:
    key = jax.random.key(seed)
    ks = jax.random.split(key, 24)
    f32 = jnp.float32
    L, D, E, F, H, C = DEPTH, D_MODEL, N_EXPERTS, D_FF, ATTN_HEADS, CONV_WIDTH

    def nrm(k, shape, fan_in, mult=1.0):
        return jax.random.normal(k, shape, f32) * (mult * fan_in ** -0.5)

    def small(k, shape, s=0.02):
        return jax.random.normal(k, shape, f32) * s

    return {
        "x": jax.random.normal(ks[0], (BATCH, SEQ, D), f32),
        "c": jax.random.normal(ks[1], (BATCH, D), f32),
        "ada_w": nrm(ks[2], (L, D, 6 * D), D, 0.5),
        "ada_b": small(ks[3], (L, 6 * D)),
        "norm_mix_g": 1.0 + small(ks[4], (L, D), 0.1),
        "norm_ffn_g": 1.0 + small(ks[5], (L, D), 0.1),
        "w_in": nrm(ks[6], (L, D, IN_COLS), D),
        "b_f": jax.random.uniform(ks[7], (L, H), f32, 1.0, 5.0),
        "q_norm_g": 1.0 + small(ks[8], (L, HEAD_DIM), 0.1),
        "k_norm_g": 1.0 + small(ks[9], (L, HEAD_DIM), 0.1),
        "conv_w": nrm(ks[10], (L, CONV_KERNEL, C), CONV_KERNEL),
        "conv_b": small(ks[11], (L, C)),
        "conv_ln_g": 1.0 + small(ks[12], (L, C), 0.1),
        "conv_ln_b": small(ks[13], (L, C)),
        "w_out": nrm(ks[14], (L, D, D), D),
        "w_router": nrm(ks[15], (L, D, E), D),
        "b_router": small(ks[16], (L, E), 0.01),
        "w_gate": nrm(ks[17], (L, E, D, F), D),
        "b_gate": small(ks[18], (L, E, F)),
        "w_up": nrm(ks[19], (L, E, D, F), D),
        "b_up": small(ks[20], (L, E, F)),
        "w_down": nrm(ks[21], (L, E, F, D), F),
        "b_down": small(ks[22], (L, E, D)),
    }


def reference(x, c, ada_w, ada_b, norm_mix_g, norm_ffn_g, w_in, b_f, q_norm_g, k_norm_g,
              conv_w, conv_b, conv_ln_g, conv_ln_b, w_out, w_router, b_router,
              w_gate, b_gate, w_up, b_up, w_down, b_down):
    c_act = jax.nn.silu(c)
    for l in range(DEPTH):
        mod = (c_act @ ada_w[l] + ada_b[l])[:, None, :]
        shift1, scale1, gate1, shift2, scale2, gate2 = jnp.split(mod, 6, axis=-1)
        h = rms_norm(x, norm_mix_g[l]) * (1 + scale1) + shift1
        x = x + gate1 * hybrid_mixer(h, w_in[l], b_f[l], q_norm_g[l], k_norm_g[l], conv_w[l], conv_b[l],
                                     conv_ln_g[l], conv_ln_b[l], w_out[l])
        h = rms_norm(x, norm_ffn_g[l]) * (1 + scale2) + shift2
        x = x + gate2 * moe_ffn(h, w_router[l], b_router[l], w_gate[l], b_gate[l], w_up[l], b_up[l],
                                w_down[l], b_down[l])
    return x
```

```python
import contextlib
import numpy as np
import ml_dtypes
import concourse.bass as bass
import concourse.mybir as mybir
from concourse.bass_utils import run_bass_kernel_spmd

F32 = mybir.dt.float32
F32R = mybir.dt.float32r
BF16 = mybir.dt.bfloat16
I32 = mybir.dt.int32
AF = mybir.ActivationFunctionType
ALU = mybir.AluOpType
AX = mybir.AxisListType

D = 2048
KD = 16
TV = 4096
NT = 32
TO = 2048
NTO = 16
NH = 8
DH = 128
CW = 1024
NG = 8
CK = 31
INC = 5128
NE = 32
CSB = 1024
TS = 256
NJ = CSB // TS
EPS = 1e-6
HALO = 256
NEGB = -30000.0

ENGS = ("sync", "act", "pool", "pe", "dve")


class Sem:
    def __init__(self, h):
        self.h = h
        self.n = 0


class KB:
    def __init__(self, nc, stack):
        self.nc = nc
        self.stack = stack
        self.q = {e: [] for e in ENGS}
        self.nsem = 0
        self.stage_sems = []

    def sem(self, name, persist=False):
        self.nsem += 1
        h = self.nc.alloc_semaphore(name=name)
        if not persist:
            self.stage_sems.append(h)
        return Sem(h)

    def end_stage(self):
        self.flush()
        if self.stage_sems:
            self.nc.all_engine_barrier()
            self.nc.clear_and_free_semaphores(self.stage_sems)
            self.nc.all_engine_barrier()
            self.stage_sems = []

    def sb(self, name, shape, dt):
        return self.stack.enter_context(self.nc.sbuf_tensor(name, list(shape), dt))

    def op(self, eng, fn, sig=None, inc=1):
        if sig is not None:
            sig.n += inc
            h = sig.h

            def f(e, fn=fn, h=h, inc=inc):
                fn(e).then_inc(h, inc)
            self.q[eng].append(f)
            return sig.n
        self.q[eng].append(lambda e, fn=fn: fn(e))
        return None

    def dma(self, eng, out, in_, sig, **kw):
        return self.op(eng, lambda e: e.dma_start(out=out, in_=in_, **kw), sig=sig, inc=16)

    def wait(self, eng, sem, val=None):
        v = sem.n if val is None else val
        if v <= 0:
            return
        self.q[eng].append(lambda e, h=sem.h, v=v: e.wait_ge(h, v))

    def raw(self, eng, fn):
        self.q[eng].append(fn)

    def flush(self):
        q = self.q
        self.q = {e: [] for e in ENGS}
        with self.nc.Block() as blk:
            @blk.sync
            def _(e):
                for f in q["sync"]:
                    f(e)

            @blk.scalar
            def _(e):
                for f in q["act"]:
                    f(e)

            @blk.gpsimd
            def _(e):
                for f in q["pool"]:
                    f(e)

            @blk.tensor
            def _(e):
                for f in q["pe"]:
                    f(e)

            @blk.vector
            def _(e):
                for f in q["dve"]:
                    f(e)


def build(stage="full", dbg_shape=None):
    nc = bass.Bass("TRN2", target_bir_lowering=False)

    def din(name, shape, dt=F32):
        return nc.dram_tensor(name, list(shape), dt, kind="ExternalInput").ap()

    def dscr(name, shape, dt=F32):
        return nc.dram_tensor(name, list(shape), dt, kind="Internal").ap()

    xv = din("xv", [TV, D])
    meta = din("meta", [128, 64])
    cT = din("cT", [128, KD])
    ada_w = din("ada_w", [D, 6 * D])
    ada_b = din("ada_b", [1, 6 * D])
    g1T = din("g1T", [128, KD])
    g2 = din("g2", [1, D])
    w_in = din("w_in", [D, INC])
    bfc = din("bfc", [8, 1])
    gq = din("gq", [128, 1])
    gk = din("gk", [128, 1])
    cwT = din("cwT", [128, NG, CK])
    cbT = din("cbT", [128, NG])
    lgT = din("lgT", [128, NG])
    lbT = din("lbT", [128, NG])
    w_out = din("w_out", [D, D])
    w_r = din("w_r", [D, NE])
    b_r = din("b_r", [1, NE])
    consts = din("consts", [128, 1024])
    sel8 = din("sel8", [8, 8 * 128])
    if stage in ("full", "moe"):
        w_g = din("w_g", [NE, D, D])
        b_g = din("b_g", [128, NE, KD])
        w_u = din("w_u", [NE, D, D])
        b_u = din("b_u", [128, NE, KD])
        w_d = din("w_d", [NE, D, D])
        b_d = din("b_d", [NE, D])
    out = nc.dram_tensor("out", [TO, D], F32, kind="ExternalOutput").ap()
    dbg = None
    if dbg_shape is not None:
        dbg = nc.dram_tensor("dbg", list(dbg_shape), F32, kind="ExternalOutput").ap()

    hTd = dscr("hTd", [KD, 128, TV])
    kTd = dscr("kTd", [NH, 128, TV], BF16)
    vd = dscr("vd", [NT, 128, NH * DH], BF16)
    qTd = dscr("qTd", [NH, 128, TO], BF16)
    u0d = dscr("u0d", [NG, 128, HALO + TO])
    x1d = dscr("x1d", [TO, D])
    xs = dscr("xs", [NE * CSB, D], BF16)
    osd = dscr("osd", [NE * CSB, D])
    cntd = dscr("cntd", [1, NE], I32)
    lTd = dscr("lTd", [8, TV])

    with contextlib.ExitStack() as stack:
        k = KB(nc, stack)
        cst = k.sb("cst", [128, 1024], F32)
        ident = cst[:, 0:128]
        tri = cst[:, 128:256]
        ustr = cst[:, 256:384]
        ones = cst[:, 384:512]
        ebase = cst[:, 512:544]
        e64 = cst[:, 544:672]
        metat = k.sb("metat", [128, 64], F32)
        identb = k.sb("identb", [128, 128], BF16)
        onesb = k.sb("onesb", [128, 128], BF16)
        gate2 = k.sb("gate2", [128, D], F32)
        gqc = k.sb("gqc", [128, 1], F32)
        gkc = k.sb("gkc", [128, 1], F32)
        w4 = k.sb("w4", [128, NTO, 4], F32)
        slot4i = k.sb("slot4i", [128, NTO, 4], I32)
        WdT = k.sb("WdT", [NE, NTO, 128], F32)
        psb = [stack.enter_context(nc.psum_tensor(f"ps{i}", [128, 512], F32)) for i in range(8)]

        s_ld = k.sem("s_ld", persist=True)
        s_c0 = k.sem("s_c0", persist=True)
        k.dma("sync", cst[:], consts, s_ld)
        k.dma("sync", metat[:], meta, s_ld)
        k.dma("sync", gqc[:], gq, s_ld)
        k.dma("sync", gkc[:], gk, s_ld)
        k.wait("dve", s_ld)
        k.op("dve", lambda e: e.tensor_copy(out=identb[:], in_=ident))
        k.op("dve", lambda e: e.tensor_scalar(out=gqc[:], in0=gqc[:], scalar1=float(DH ** -0.5), scalar2=None, op0=ALU.mult))
        k.op("dve", lambda e: e.tensor_copy(out=onesb[:], in_=ones), sig=s_c0)

        ctx = dict(locals())
        ctx["k"] = k
        ctx["stack"] = stack
        with contextlib.ExitStack() as sA:
            ctx["modbc"] = sA.enter_context(nc.sbuf_tensor("modbc", [128, 3, D], F32))
            ctx["a1c"] = sA.enter_context(nc.sbuf_tensor("a1c", [128, KD], F32))
            ctx["b1c"] = sA.enter_context(nc.sbuf_tensor("b1c", [128, KD], F32))
            stage_mod(ctx)
            stage_ht(ctx)
            if stage == "s1":
                finish_dbg(ctx, "s1")
                return nc
            with contextlib.ExitStack() as sB:
                stage_proj(ctx)
                if stage == "s2":
                    finish_dbg(ctx, "s2")
                    return nc
                with contextlib.ExitStack() as sC:
                    ctx["conv_outT"] = sC.enter_context(nc.sbuf_tensor("conv_outT", [128, NG, TO], BF16))
                    ctx["attn_outT"] = sC.enter_context(nc.sbuf_tensor("attn_outT", [128, NH, TO], BF16))
                    stage_conv(ctx)
                    if stage == "s3":
                        finish_dbg(ctx, "s3")
                        return nc
                    stage_attn(ctx)
                    if stage == "s4":
                        finish_dbg(ctx, "s4")
                        return nc
                    stage_outproj(ctx)
            stage_route(ctx)
            if stage == "s6":
                finish_dbg(ctx, "s6")
                return nc
        stage_moe(ctx)
        stage_combine(ctx)
    return nc


def stage_mod(c):
    k, nc, stack, psb = c["k"], c["nc"], c["stack"], c["psb"]
    cT, ada_w, ada_b, g1T, g2 = c["cT"], c["ada_w"], c["ada_b"], c["g1T"], c["g2"]
    modbc, a1c, b1c, ident, ones = c["modbc"], c["a1c"], c["b1c"], c["ident"], c["ones"]
    with contextlib.ExitStack() as st:
        ct = st.enter_context(nc.sbuf_tensor("m_ct", [128, KD], F32))
        sg = st.enter_context(nc.sbuf_tensor("m_sg", [128, KD], F32))
        cbc = st.enter_context(nc.sbuf_tensor("m_cbc", [128, KD, 128], F32))
        abt = st.enter_context(nc.sbuf_tensor("m_ab", [1, 6 * D], F32))
        g1t = st.enter_context(nc.sbuf_tensor("m_g1", [128, KD], F32))
        g2b = st.enter_context(nc.sbuf_tensor("m_g2b", [128, D], F32))
        wbuf = [st.enter_context(nc.sbuf_tensor(f"m_w{i}", [128, KD, 512], F32)) for i in range(2)]
        ss1 = st.enter_context(nc.sbuf_tensor("m_ss1", [128, 2, D], F32))
        tmp = st.enter_context(nc.sbuf_tensor("m_tmp", [128, KD, 128], F32))
        zt = st.enter_context(nc.sbuf_tensor("m_zt", [128, D], BF16))
        s_z0 = k.sem("m_z0")
        s_z = k.sem("zfill", persist=True)
        c["s_z"] = s_z
        k.op("dve", lambda e: e.memset(zt[:], 0.0), sig=s_z0)
        k.wait("pool", s_z0)
        xs_ = c["xs"]
        zv = k.dma("pool", xs_[0:128, :], zt[:], s_z)
        zfirst = zv
        nz = 128
        while nz < NE * CSB:
            k.wait("pool", s_z, zv)
            step = min(nz, 2048)
            for r0 in range(nz, min(2 * nz, NE * CSB), step):
                zv = k.dma("pool", xs_[r0:r0 + step, :], xs_[0:step, :], s_z)
            nz *= 2
        s_in = k.sem("m_in")
        s_w = [k.sem("m_w0"), k.sem("m_w1")]
        s_fr = k.sem("m_fr")
        s_pe = k.sem("m_pe")
        s_ev = k.sem("m_ev")
        s_a = k.sem("m_a")
        s_v = k.sem("m_v")
        k.dma("sync", ct[:], cT, s_in)
        k.dma("sync", abt[:], ada_b, s_in)
        k.dma("sync", g1t[:], g1T, s_in)
        k.dma("sync", g2b[:], g2.partition_broadcast(128), s_in)
        k.wait("act", s_in)
        k.op("act", lambda e: e.activation(out=sg[:], in_=ct[:], func=AF.Sigmoid), sig=s_a)
        k.wait("dve", s_a)
        k.wait("dve", s_in)
        k.op("dve", lambda e: e.tensor_mul(out=sg[:], in0=sg[:], in1=ct[:]), sig=s_v)
        k.wait("dve", s_v)
        vb = k.op("dve", lambda e: e.tensor_copy(out=cbc[:], in_=sg[:].unsqueeze(2).to_broadcast([128, KD, 128])), sig=s_v)
        k.wait("pe", s_v, vb)
        k.wait("pe", s_in)
        k.wait("pe", c["s_ld"])
        NGp = 24
        for gi in range(NGp):
            b = gi % 2
            if gi >= 2:
                k.wait("sync", s_fr, gi - 1)
            src = ada_w[:, gi * 512:(gi + 1) * 512].rearrange("(kk p) n -> p kk n", p=128)
            wv = k.dma("sync", wbuf[b][:], src, s_w[b])
            k.wait("pe", s_w[b], wv)
            ps = psb[gi % 4]
            if gi >= 4:
                k.wait("pe", s_ev, gi - 3)
            for kk in range(KD):
                k.op("pe", lambda e, ps=ps, kk=kk, b=b: e.matmul(ps[:, :], lhsT=cbc[:, kk, :], rhs=wbuf[b][:, kk, :], start=(kk == 0), stop=False),
                     sig=(s_fr if kk == KD - 1 else None))
            pv = k.op("pe", lambda e, ps=ps, gi=gi: e.matmul(ps[:, :], lhsT=ones[0:1, :], rhs=abt[0:1, gi * 512:(gi + 1) * 512], start=False, stop=True), sig=s_pe)
            k.wait("act", s_pe, pv)
            which, sub = gi // 4, gi % 4
            if which in (0, 1):
                dst = ss1[:, which, sub * 512:(sub + 1) * 512]
            elif which == 2:
                dst = modbc[:, 0, sub * 512:(sub + 1) * 512]
            elif which == 3:
                dst = modbc[:, 2, sub * 512:(sub + 1) * 512]
            elif which == 4:
                dst = modbc[:, 1, sub * 512:(sub + 1) * 512]
            else:
                dst = c["gate2"][:, sub * 512:(sub + 1) * 512]
            k.op("act", lambda e, dst=dst, ps=ps: e.activation(out=dst, in_=ps[:, :], func=AF.Copy), sig=s_ev)
        k.wait("dve", s_ev)
        s_d = k.sem("m_d")
        for which, dstc in ((0, b1c), (1, a1c)):
            v1 = k.op("dve", lambda e, which=which: e.tensor_tensor(
                out=tmp[:], in0=ss1[:, which, :].rearrange("p (a b) -> p a b", b=128),
                in1=ident.unsqueeze(1).to_broadcast([128, KD, 128]), op=ALU.mult), sig=s_d)
            k.wait("dve", s_d, v1)
            v2 = k.op("dve", lambda e, dstc=dstc: e.tensor_reduce(out=dstc[:], in_=tmp[:], axis=AX.X, op=ALU.add), sig=s_d)
            k.wait("dve", s_d, v2)
        v3 = k.op("dve", lambda e: e.scalar_tensor_tensor(out=a1c[:], in0=a1c[:], scalar=1.0, in1=g1t[:], op0=ALU.add, op1=ALU.mult), sig=s_d)
        v4 = k.op("dve", lambda e: e.scalar_tensor_tensor(out=modbc[:, 1, :], in0=modbc[:, 1, :], scalar=1.0, in1=g2b[:], op0=ALU.add, op1=ALU.mult), sig=s_d)
        c["s_mod"] = s_d
        for eng in ENGS:
            k.wait(eng, s_d)
        k.wait("pool", s_z, zfirst)
        k.end_stage()


def stage_ht(c):
    k, nc, psb = c["k"], c["nc"], c["psb"]
    xv, hTd, a1c, b1c, ident = c["xv"], c["hTd"], c["a1c"], c["b1c"], c["ident"]
    with contextlib.ExitStack() as st:
        xt = [st.enter_context(nc.sbuf_tensor(f"h_x{i}", [128, D], F32)) for i in range(2)]
        xn = [st.enter_context(nc.sbuf_tensor(f"h_xn{i}", [128, D], F32)) for i in range(2)]
        junk = st.enter_context(nc.sbuf_tensor("h_junk", [128, D], F32))
        ssq = st.enter_context(nc.sbuf_tensor("h_ssq", [128, NT], F32))
        rstd = st.enter_context(nc.sbuf_tensor("h_rstd", [128, NT], F32))
        hs = [st.enter_context(nc.sbuf_tensor(f"h_hs{i}", [128, KD, 512], F32)) for i in range(2)]
        s_x = [k.sem("h_x0"), k.sem("h_x1")]
        s_sq = k.sem("h_sq")
        s_rs = k.sem("h_rs")
        s_xn = k.sem("h_xn")
        s_tp = k.sem("h_tp")
        s_eva = k.sem("h_eva")
        s_evv = k.sem("h_evv")
        s_st = [k.sem("h_st0"), k.sem("h_st1")]
        for i in range(NT):
            b = i % 2
            if i >= 2:
                k.wait("sync", s_xn, i - 1)
            xvv = k.dma("sync", xt[b][:], xv[i * 128:(i + 1) * 128, :], s_x[b])
            k.wait("act", s_x[b], xvv)
            sv = k.op("act", lambda e, b=b, i=i: e.activation(out=junk[:], in_=xt[b][:], func=AF.Square, accum_out=ssq[:, i:i + 1]), sig=s_sq)
            k.wait("dve", s_sq, sv)
            r1 = k.op("dve", lambda e, i=i: e.tensor_scalar(out=rstd[:, i:i + 1], in0=ssq[:, i:i + 1], scalar1=1.0 / D, scalar2=EPS, op0=ALU.mult, op1=ALU.add), sig=s_rs)
            k.wait("act", s_rs, r1)
            rq = k.op("act", lambda e, i=i: e.activation(out=rstd[:, i:i + 1], in_=rstd[:, i:i + 1], func=AF.Sqrt), sig=s_sq)
            k.wait("dve", s_sq, rq)
            r2 = k.op("dve", lambda e, i=i: e.reciprocal(out=rstd[:, i:i + 1], in_=rstd[:, i:i + 1]), sig=s_rs)
            k.wait("act", s_rs, r2)
            if i >= 2:
                k.wait("act", s_tp, 4 * (i - 1))
            nv = k.op("act", lambda e, b=b, i=i: e.activation(out=xn[b][:], in_=xt[b][:], func=AF.Identity, scale=rstd[:, i:i + 1]), sig=s_xn)
            k.wait("pe", s_xn, nv)
            hb = (i // 4) % 2
            sub = i % 4
            ch = i // 4
            if sub == 0 and ch >= 2:
                k.wait("act", s_st[hb], 16 * (ch // 2))
                k.wait("dve", s_st[hb], 16 * (ch // 2))
            for q4 in range(4):
                ps = psb[4 * (i % 2) + q4]
                ev_eng = "act" if q4 % 2 == 0 else "dve"
                s_ev = s_eva if ev_eng == "act" else s_evv
                if i >= 2:
                    k.wait("pe", s_ev, 2 * (i - 2) + q4 // 2 + 1)
                for kk4 in range(4):
                    kk = 4 * q4 + kk4
                    k.op("pe", lambda e, ps=ps, kk=kk, kk4=kk4, b=b: e.transpose(ps[:, kk4 * 128:(kk4 + 1) * 128], xn[b][:, kk * 128:(kk + 1) * 128], ident),
                         sig=(s_tp if kk4 == 3 else None))
                k.wait(ev_eng, s_tp)
                for kk4 in range(4):
                    kk = 4 * q4 + kk4
                    dst = hs[hb][:, kk, sub * 128:(sub + 1) * 128]
                    src = ps[:, kk4 * 128:(kk4 + 1) * 128]
                    sg = (s_ev if kk4 == 3 else None)
                    if ev_eng == "act":
                        k.op("act", lambda e, dst=dst, src=src, kk=kk: e.activation(out=dst, in_=src, func=AF.Identity, scale=a1c[:, kk:kk + 1], bias=b1c[:, kk:kk + 1]), sig=sg)
                    else:
                        k.op("dve", lambda e, dst=dst, src=src, kk=kk: e.tensor_scalar(out=dst, in0=src, scalar1=a1c[:, kk:kk + 1], scalar2=b1c[:, kk:kk + 1], op0=ALU.mult, op1=ALU.add), sig=sg)
            if sub == 3:
                k.wait("sync", s_eva)
                k.wait("sync", s_evv)
                k.dma("sync", hTd[:, :, ch * 512:(ch + 1) * 512].rearrange("kk p t -> p kk t"), hs[hb][:], s_st[hb])
        for eng in ENGS:
            k.wait(eng, s_st[0])
            k.wait(eng, s_st[1])
        k.end_stage()


def finish_dbg(c, what):
    k, nc = c["k"], c["nc"]
    dbg = c["dbg"]
    s_o = k.sem("dbg_o")
    if what == "s1":
        k.dma("sync", dbg[0:KD, :, :], c["hTd"][:, :, 2048:2560], s_o)
        k.dma("sync", dbg[KD:KD + 1, 0:16, :].rearrange("o (a b) c -> o a (b c)", a=4), c["modbc"][0:1, :, :], s_o)
    if what == "s2":
        k.dma("pool", dbg[0, :, :], c["kTd"][3, :, :], s_o)
        k.dma("pool", dbg[1, :, 0:TO], c["qTd"][5, :, :], s_o)
        k.dma("pool", dbg[2, :, 0:NH * DH], c["vd"][20, :, :], s_o)
        k.dma("pool", dbg[3, :, 0:HALO + TO], c["u0d"][2, :, :], s_o)
        k.dma("pool", dbg[4, 0:8, :], c["lTd"], s_o)
        k.wait("pool", s_o)
    if what == "s3":
        k.dma("pool", dbg[0], c["conv_outT"][:, :, :], s_o)
        k.wait("pool", s_o)
    if what == "s4":
        k.dma("pool", dbg[0], c["conv_outT"][:, :, :], s_o)
        k.dma("pool", dbg[1], c["attn_outT"][:, :, :], s_o)
        k.wait("pool", s_o)
    if what == "s6":
        k.dma("sync", dbg[0], c["x1d"], s_o)
        k.dma("pool", dbg[1, 0:128, 0:64], c["w4"][:, :, :].rearrange("p a b -> p (a b)"), s_o)
        k.dma("pool", dbg[1, 128:256, 0:64], c["slot4i"][:, :, :].rearrange("p a b -> p (a b)"), s_o)
        k.dma("pool", dbg[1, 256:257, 0:32], c["cntd"], s_o)
        k.wait("pool", s_o)
    k.wait("sync", s_o)
    k.flush()


def make_consts():
    cst = np.zeros((128, 1024), np.float32)
    p = np.arange(128)
    cst[:, 0:128] = np.eye(128, dtype=np.float32)
    cst[:, 128:256] = (p[:, None] <= p[None, :]).astype(np.float32)
    cst[:, 256:384] = (p[:, None] < p[None, :]).astype(np.float32)
    cst[:, 384:512] = 1.0
    cst[:, 512:544] = (np.arange(NE) * CSB)[None, :].astype(np.float32)
    cst[64, 544:672] = 1.0
    sel8 = np.zeros((8, 8 * 128), np.float32)
    for h in range(8):
        sel8[h, h * 128:(h + 1) * 128] = 1.0
    return cst, sel8


def prep_inputs(inp, with_moe=True):
    f = lambda a: np.ascontiguousarray(np.asarray(a, dtype=np.float32))
    x = f(inp["x"])
    cst, sel8 = make_consts()
    shared = {
        "ada_w": f(inp["ada_w"][0]), "ada_b": f(inp["ada_b"][0]).reshape(1, -1),
        "g1T": f(np.asarray(inp["norm_mix_g"][0]).reshape(KD, 128).T),
        "g2": f(inp["norm_ffn_g"][0]).reshape(1, D),
        "w_in": f(inp["w_in"][0]),
        "bfc": f(inp["b_f"][0]).reshape(8, 1),
        "gq": f(inp["q_norm_g"][0]).reshape(128, 1), "gk": f(inp["k_norm_g"][0]).reshape(128, 1),
        "cwT": f(np.asarray(inp["conv_w"][0]).reshape(CK, NG, 128).transpose(2, 1, 0)),
        "cbT": f(np.asarray(inp["conv_b"][0]).reshape(NG, 128).T),
        "lgT": f(np.asarray(inp["conv_ln_g"][0]).reshape(NG, 128).T),
        "lbT": f(np.asarray(inp["conv_ln_b"][0]).reshape(NG, 128).T),
        "w_out": f(inp["w_out"][0]), "w_r": f(inp["w_router"][0]), "b_r": f(inp["b_router"][0]).reshape(1, NE),
        "consts": cst, "sel8": sel8,
    }
    if with_moe:
        shared.update({
            "w_g": f(inp["w_gate"][0]), "w_u": f(inp["w_up"][0]), "w_d": f(inp["w_down"][0]),
            "b_g": f(np.asarray(inp["b_gate"][0]).reshape(NE, KD, 128).transpose(2, 0, 1)),
            "b_u": f(np.asarray(inp["b_up"][0]).reshape(NE, KD, 128).transpose(2, 0, 1)),
            "b_d": f(inp["b_down"][0]),
        })
    maps = []
    for core in range(8):
        b, s = core // 2, core % 2
        meta = np.zeros((128, 64), np.float32)
        if s == 0:
            xvv = np.concatenate([np.zeros((TO, D), np.float32), x[b, :TO]], axis=0)
            meta[:, 0:16] = NEGB
            meta[:, 32] = 0.0
        else:
            xvv = x[b]
            meta[:, 32] = 1.0
        m = dict(shared)
        m["xv"] = np.ascontiguousarray(xvv)
        m["meta"] = meta
        m["cT"] = f(np.asarray(inp["c"][b]).reshape(KD, 128).T)
        maps.append(m)
    return maps


def _input_names(nc):
    return [a.memorylocations[0].name for a in nc.allocations
            if isinstance(a, mybir.MemoryLocationSet) and a.kind == "ExternalInput"]


def kernel(**inputs):
    maps = prep_inputs(inputs, with_moe=True)
    nc = build("full")
    names = _input_names(nc)
    maps = [{n: m[n] for n in names if n in m} for m in maps]
    res = run_bass_kernel_spmd(nc, maps, core_ids=list(range(8)))
    out = np.empty((4, TV, D), np.float32)
    for core in range(8):
        b, s = core // 2, core % 2
        out[b, s * TO:(s + 1) * TO] = res.results[core]["out"]
    return out


class Ring:
    def __init__(self, k, name, n):
        self.k, self.n, self.i, self.name = k, n, 0, name
        self.rel = k.sem(name)
        self.vals = [0] * n
        self.dsem = [None] * n
        self.dvals = [0] * n

    def next(self, *engs):
        slot = self.i % self.n
        for e in engs:
            if self.vals[slot]:
                self.k.wait(e, self.rel, self.vals[slot])
            if self.dvals[slot]:
                self.k.wait(e, self.dsem[slot], self.dvals[slot])
        self.i += 1
        return slot

    def release(self, slot, eng, fn):
        self.vals[slot] = self.k.op(eng, fn, sig=self.rel)
        return self.vals[slot]

    def release_dma(self, slot, eng, out, in_):
        if self.dsem[slot] is None:
            self.dsem[slot] = self.k.sem(f"{self.name}_d{slot}")
        self.dvals[slot] = self.k.dma(eng, out, in_, self.dsem[slot])
        return self.dvals[slot]

    def wait_all(self, eng):
        self.k.wait(eng, self.rel)
        for sl in range(self.n):
            if self.dsem[sl] is not None:
                self.k.wait(eng, self.dsem[sl])


def stage_proj(c):
    k, nc, psb = c["k"], c["nc"], c["psb"]
    hTd, w_in, kTd, qTd, vd, u0d = c["hTd"], c["w_in"], c["kTd"], c["qTd"], c["vd"], c["u0d"]
    ones, metat, gqc, gkc = c["ones"], c["metat"], c["gqc"], c["gkc"]
    lTd = c["lTd"]
    print("sbuf before proj", nc.sbuf_bytes_remaining)
    with contextlib.ExitStack() as st:
        sbt = lambda n, s, d: st.enter_context(nc.sbuf_tensor(n, list(s), d))
        lch = [sbt(f"p_l{i}", [8, 512], F32) for i in range(2)]
        r_l = Ring(k, "p_rl", 2)
        htc = [sbt(f"p_ht{i}", [128, KD, 512], F32R) for i in range(2)]
        wp = [sbt(f"p_wp{i}", [128, KD, 256], F32R) for i in range(3)]
        onesr = sbt("p_onesr", [128, 128], F32R)
        epsc = sbt("p_eps", [128, 1], F32)
        nbf = sbt("p_nbf", [8, 1], F32)
        nbf2 = sbt("p_nbf2", [8, 1], F32)
        sqb = [sbt(f"p_sq{i}", [128, 512], F32R) for i in range(2)]
        lnb = [sbt(f"p_ln{i}", [128, 512], F32) for i in range(2)]
        knb = [sbt(f"p_kn{i}", [128, 512], BF16) for i in range(2)]
        vst = [sbt(f"p_vs{i}", [128, 4, NH * DH], BF16) for i in range(1)]
        sgb = [sbt(f"p_sg{i}", [128, 512], F32) for i in range(2)]
        u0s = [sbt(f"p_u0{i}", [128, 512], F32) for i in range(2)]
        fe = sbt("p_fe", [8, 512], F32)

        s_i = k.sem("p_i")
        k.dma("sync", nbf[:], c["bfc"], s_i)
        k.wait("dve", s_i)
        k.op("dve", lambda e: e.tensor_copy(out=onesr[:], in_=ones))
        k.op("dve", lambda e: e.memset(epsc[:], EPS))
        k.op("dve", lambda e: e.tensor_scalar(out=nbf2[:], in0=nbf[:], scalar1=-1.0, scalar2=None, op0=ALU.mult), sig=s_i)
        for eng in ("pe", "act"):
            k.wait(eng, s_i)
        k.wait("pe", c["s_c0"])

        s_ht = [k.sem("p_ht0"), k.sem("p_ht1")]
        s_wp = [k.sem(f"p_wp{i}") for i in range(3)]
        r_ht = Ring(k, "p_rht", 2)
        r_wp = Ring(k, "p_rwp", 3)
        r_main = Ring(k, "p_rmain", 4)
        r_ss = Ring(k, "p_rss", 2)
        r_sq = Ring(k, "p_rsq", 2)
        r_ln = Ring(k, "p_rln", 2)
        r_kn = Ring(k, "p_rkn", 2)
        r_vs = Ring(k, "p_rvs", 1)
        r_sg = Ring(k, "p_rsg", 2)
        r_u0 = Ring(k, "p_ru0", 2)
        r_f = Ring(k, "p_rf", 1)
        s_pe = k.sem("p_pe")
        s_act = k.sem("p_act")
        s_dve = k.sem("p_dve")
        s_pe2 = k.sem("p_pe2")
        s_act2 = k.sem("p_act2")

        onesb_ = c["onesb"]

        def pe_dummy(e):
            return e.matmul(psb[7][0:1, 0:2], lhsT=onesb_[:, 0:1], rhs=onesb_[:, 0:2], start=True, stop=True)

        pending = []

        def flush_pending():
            while pending:
                pending.pop(0)()

        def load_piece(col_ranges):
            slot = r_wp.next("pool")
            off = 0
            for (c0, c1) in col_ranges:
                v = k.dma("pool", wp[slot][:, :, off:off + (c1 - c0)], w_in[:, c0:c1].rearrange("(kk p) n -> p kk n", p=128), s_wp[slot])
                off += c1 - c0
            k.wait("pe", s_wp[slot], v)
            return slot

        def qk_unit(hslot, wslot, wcol, ntok, gcol, dst_ap):
            bank = r_main.next("pe")
            ps = psb[bank]
            for kk in range(KD):
                k.op("pe", lambda e, kk=kk: e.matmul(ps[:, :ntok], lhsT=wp[wslot][:, kk, wcol:wcol + 128], rhs=htc[hslot][:, kk, 512 - ntok:512], start=(kk == 0), stop=(kk == KD - 1)),
                     sig=(s_pe if kk == KD - 1 else None))
            pv = s_pe.n
            sq = r_sq.next("act")
            k.wait("act", s_pe, pv)
            av = k.op("act", lambda e: e.activation(out=sqb[sq][:, :ntok], in_=ps[:, :ntok], func=AF.Square), sig=s_act)

            def ssum_part():
                sb_ = r_ss.next("pe")
                pss = psb[4 + sb_]
                k.wait("pe", s_act, av)
                sv = r_sq.release(sq, "pe", lambda e: e.matmul(pss[:, :ntok], lhsT=onesr[:, :], rhs=sqb[sq][:, :ntok], start=True, stop=True))
                ln = r_ln.next("act")
                k.wait("act", r_sq.rel, sv)
                a1 = k.op("act", lambda e: e.activation(out=lnb[ln][:, :ntok], in_=pss[:, :ntok], func=AF.Ln, scale=1.0 / DH, bias=epsc[:, 0:1]), sig=s_act2)
                k.wait("act", s_act2, a1)
                a2 = r_ss.release(sb_, "act", lambda e: e.activation(out=lnb[ln][:, :ntok], in_=lnb[ln][:, :ntok], func=AF.Exp, scale=-0.5))
                kn = r_kn.next("dve")
                k.wait("dve", r_ss.rel, a2)
                dv = r_main.release(bank, "dve", lambda e: e.scalar_tensor_tensor(out=knb[kn][:, :ntok], in0=ps[:, :ntok], scalar=gcol[:, 0:1], in1=lnb[ln][:, :ntok], op0=ALU.mult, op1=ALU.mult))
                k.wait("dve", r_main.rel, dv)
                r_ln.release(ln, "dve", lambda e: e.tensor_copy(out=lnb[ln][0:1, 0:1], in_=lnb[ln][0:1, 0:1]))
                k.wait("sync", r_main.rel, dv)
                r_kn.release_dma(kn, "sync", dst_ap, knb[kn][:, :ntok])
            flush_pending()
            pending.append(ssum_part)

        for ci in range(8):
            own = ci >= 4
            hslot = r_ht.next("pool")
            hv_ = k.dma("pool", htc[hslot][:], hTd[:, :, ci * 512:(ci + 1) * 512].rearrange("kk p t -> p kk t"), s_ht[hslot])
            k.wait("pe", s_ht[hslot], hv_)
            last_pe_user = []
            jobs = [("k", i) for i in range(4)] + ([("q", i) for i in range(4)] if own else [])
            for kind, i in jobs:
                base = 1024 if kind == "k" else 0
                wslot = load_piece([(base + 256 * i, base + 256 * (i + 1))])
                for hh in range(2):
                    h = 2 * i + hh
                    if kind == "k":
                        dst = kTd[h, :, ci * 512:(ci + 1) * 512]
                        gcol = gkc
                    else:
                        dst = qTd[h, :, (ci - 4) * 512:(ci - 3) * 512]
                        gcol = gqc
                    qk_unit(hslot, wslot, 128 * hh, 512, gcol, dst)
                r_wp.release(wslot, "pe", pe_dummy)
            flush_pending()
            r_f.next("pe")
            wslot = load_piece([(3072, 3200)])
            for kk in range(KD):
                k.op("pe", lambda e, kk=kk, hslot=hslot, wslot=wslot: e.matmul(psb[6][:, :], lhsT=wp[wslot][:, kk, 0:128], rhs=htc[hslot][:, kk, :], start=(kk == 0), stop=(kk == KD - 1)),
                     sig=(s_pe if kk == KD - 1 else None))
            r_wp.release(wslot, "pe", pe_dummy)
            k.wait("act", s_pe)
            fa = k.op("act", lambda e: e.activation(out=fe[:, :], in_=psb[6][0:8, :], func=AF.Exp, scale=-1.0, bias=nbf2[:, 0:1]), sig=s_act2)
            k.wait("act", s_act2, fa)
            li = r_l.next("act")
            lv = r_f.release(0, "act", lambda e, li=li: e.activation(out=lch[li][:, :], in_=fe[:, :], func=AF.Ln, scale=1.0, bias=ones[0:8, 0:1]))
            k.wait("sync", r_f.rel, lv)
            r_l.release_dma(li, "sync", lTd[:, ci * 512:(ci + 1) * 512], lch[li][:, :])
            vs = r_vs.next("act", "dve")
            for i in range(4):
                wslot = load_piece([(2048 + 256 * i, 2048 + 256 * (i + 1))])
                for tt in range(4):
                    bank = r_main.next("pe")
                    ps = psb[bank]
                    for kk in range(KD):
                        k.op("pe", lambda e, kk=kk, tt=tt, ps=ps, wslot=wslot, hslot=hslot: e.matmul(ps[:, 0:256], lhsT=htc[hslot][:, kk, tt * 128:(tt + 1) * 128], rhs=wp[wslot][:, kk, :], start=(kk == 0), stop=(kk == KD - 1)),
                             sig=(s_pe if kk == KD - 1 else None))
                    k.wait("dve", s_pe)
                    r_main.release(bank, "dve", lambda e, ps=ps, tt=tt, i=i, vs=vs: e.tensor_copy(out=vst[vs][:, tt, 256 * i:256 * (i + 1)], in_=ps[:, 0:256]))
                r_wp.release(wslot, "pe", pe_dummy)
            k.wait("sync", r_main.rel)
            r_vs.release_dma(vs, "sync", vd[4 * ci:4 * ci + 4, :, :].rearrange("t p n -> p t n"), vst[vs][:])
            if ci >= 3:
                ntok = 512 if own else HALO
                ucol = (HALO + (ci - 4) * 512) if own else 0
                for g in range(NG):
                    wslot = load_piece([(3080 + 128 * g, 3080 + 128 * (g + 1)), (4104 + 128 * g, 4104 + 128 * (g + 1))])
                    banks = []
                    for part in range(2):
                        bank = r_main.next("pe")
                        banks.append(bank)
                        ps = psb[bank]
                        for kk in range(KD):
                            k.op("pe", lambda e, kk=kk, ps=ps, part=part, wslot=wslot, hslot=hslot, ntok=ntok: e.matmul(ps[:, :ntok], lhsT=wp[wslot][:, kk, 128 * part:128 * (part + 1)], rhs=htc[hslot][:, kk, 512 - ntok:512], start=(kk == 0), stop=(kk == KD - 1)),
                                 sig=(s_pe if kk == KD - 1 else None))
                    r_wp.release(wslot, "pe", pe_dummy)
                    pa, pg = psb[banks[0]], psb[banks[1]]
                    sgi = r_sg.next("act")
                    k.wait("act", s_pe)
                    sv = k.op("act", lambda e, sgi=sgi, pg=pg, ntok=ntok: e.activation(out=sgb[sgi][:, :ntok], in_=pg[:, :ntok], func=AF.Sigmoid), sig=s_act)
                    ui = r_u0.next("dve")
                    k.wait("dve", s_act, sv)
                    if own:
                        k.op("dve", lambda e, ui=ui, pa=pa, sgi=sgi, ntok=ntok: e.tensor_tensor(out=u0s[ui][:, :ntok], in0=pa[:, :ntok], in1=sgb[sgi][:, :ntok], op=ALU.mult), sig=s_dve)
                    else:
                        k.op("dve", lambda e, ui=ui, pa=pa, sgi=sgi, ntok=ntok: e.scalar_tensor_tensor(out=u0s[ui][:, :ntok], in0=pa[:, :ntok], scalar=metat[:, 32:33], in1=sgb[sgi][:, :ntok], op0=ALU.mult, op1=ALU.mult), sig=s_dve)
                    dv = s_dve.n
                    k.wait("dve", s_dve, dv)
                    r_main.release(banks[0], "dve", lambda e, sgi=sgi: e.tensor_copy(out=sgb[sgi][0:1, 0:1], in_=sgb[sgi][0:1, 0:1]))
                    r_main.release(banks[1], "dve", lambda e, sgi=sgi: e.tensor_copy(out=sgb[sgi][0:1, 1:2], in_=sgb[sgi][0:1, 1:2]))
                    r_sg.release(sgi, "dve", lambda e, sgi=sgi: e.tensor_copy(out=sgb[sgi][0:1, 2:3], in_=sgb[sgi][0:1, 2:3]))
                    k.wait("sync", s_dve, dv)
                    r_u0.release_dma(ui, "sync", u0d[g, :, ucol:ucol + ntok], u0s[ui][:, :ntok])
            r_ht.release(hslot, "pe", pe_dummy)
        fin = k.sem("p_fin")
        for r in (r_kn, r_vs, r_u0, r_l):
            r.wait_all("sync")
        k.wait("sync", r_f.rel)
        k.op("sync", lambda e: e.dma_start(out=c["cntd"][0:1, 0:1], in_=c["cntd"][0:1, 1:2]), sig=fin, inc=16)
        for eng in ENGS:
            k.wait(eng, fin)
        k.end_stage()


def stage_conv(c):
    print("sbuf before conv", c["nc"].sbuf_bytes_remaining)
    k, nc, psb = c["k"], c["nc"], c["psb"]
    u0d, cwT, cbT, lgT, lbT, ones = c["u0d"], c["cwT"], c["cbT"], c["lgT"], c["lbT"], c["ones"]
    conv_outT = c["conv_outT"]
    with contextlib.ExitStack() as st:
        sbt = lambda n, s, d: st.enter_context(nc.sbuf_tensor(n, list(s), d))
        u0 = [sbt(f"c_u{i}", [128, HALO + TO], F32) for i in range(2)]
        v = sbt("c_v", [128, NG, TO], F32)
        cw = sbt("c_cw", [128, NG, CK], F32)
        cb = sbt("c_cb", [128, NG], F32)
        lg = sbt("c_lg", [128, NG], F32)
        lb = sbt("c_lb", [128, NG], F32)
        epsc = sbt("c_eps", [128, 1], F32)
        sqt = [sbt(f"c_sq{i}", [128, 512], F32) for i in range(2)]
        mean = sbt("c_mean", [128, 512], F32)
        msq = sbt("c_msq", [128, 512], F32)
        rstd = sbt("c_rstd", [128, 512], F32)
        yt = [sbt(f"c_y{i}", [128, 512], F32) for i in range(2)]
        s_i = k.sem("c_i")
        k.dma("sync", cw[:], cwT, s_i)
        k.dma("sync", cb[:], cbT, s_i)
        k.dma("sync", lg[:], lgT, s_i)
        k.dma("sync", lb[:], lbT, s_i)
        s_u = [k.sem("c_u0"), k.sem("c_u1")]
        s_cv = k.sem("c_cvd")
        r_u = Ring(k, "c_rud", 2)
        k.op("dve", lambda e: e.memset(epsc[:], EPS))
        for eng in ("dve", "pool", "act"):
            k.wait(eng, s_i)
        gdone = {}
        eng = "dve"
        for g in range(NG):
            b = r_u.next("sync")
            uv = k.dma("sync", u0[b][:], u0d[g, :, :], s_u[b])
            k.wait(eng, s_u[b], uv)
            sc = s_cv
            base = HALO - (CK - 1)
            vv = k.op(eng, lambda e, g=g, b=b: e.tensor_scalar(out=v[:, g, :], in0=u0[b][:, base:base + TO], scalar1=cw[:, g, 0:1], scalar2=cb[:, g:g + 1], op0=ALU.mult, op1=ALU.add), sig=sc)
            for t in range(1, CK):
                k.wait(eng, sc, vv)
                if t < CK - 1:
                    vv = k.op(eng, lambda e, g=g, b=b, t=t: e.scalar_tensor_tensor(out=v[:, g, :], in0=u0[b][:, base + t:base + t + TO], scalar=cw[:, g, t:t + 1], in1=v[:, g, :], op0=ALU.mult, op1=ALU.add), sig=sc)
                else:
                    vv = r_u.release(b, eng, lambda e, g=g, b=b, t=t: e.scalar_tensor_tensor(out=v[:, g, :], in0=u0[b][:, base + t:base + t + TO], scalar=cw[:, g, t:t + 1], in1=v[:, g, :], op0=ALU.mult, op1=ALU.add))
            gdone[g] = (r_u.rel, vv)
        for (sm, vv) in gdone.values():
            for eng in ("pe", "act", "dve"):
                k.wait(eng, sm, vv)
        s_sq = k.sem("c_sq")
        r_sq = Ring(k, "c_rsq", 2)
        s_pe = k.sem("c_pe")
        s_d = k.sem("c_d")
        s_a = k.sem("c_a")
        r_y = Ring(k, "c_ry", 2)
        s_fin = k.sem("c_fin")
        for tc in range(4):
            cs = slice(tc * 512, (tc + 1) * 512)
            ps1, ps2 = psb[2 * (tc % 2)], psb[2 * (tc % 2) + 1]
            if tc >= 2:
                k.wait("pe", s_d, dfree[tc - 2])
            for g in range(NG):
                k.op("pe", lambda e, g=g, ps1=ps1, cs=cs: e.matmul(ps1[:, :], lhsT=ones, rhs=v[:, g, cs], start=(g == 0), stop=(g == NG - 1)))
            for g in range(NG):
                sq = r_sq.next("act")
                av = k.op("act", lambda e, g=g, sq=sq, cs=cs: e.activation(out=sqt[sq][:, :], in_=v[:, g, cs], func=AF.Square), sig=s_sq)
                k.wait("pe", s_sq, av)
                r_sq.release(sq, "pe", lambda e, g=g, sq=sq, ps2=ps2: e.matmul(ps2[:, :], lhsT=ones, rhs=sqt[sq][:, :], start=(g == 0), stop=(g == NG - 1)))
            pv = r_sq.rel.n
            k.wait("dve", r_sq.rel, pv)
            if tc >= 1:
                k.wait("dve", r_y.rel)
            d1 = k.op("dve", lambda e, ps1=ps1: e.tensor_scalar(out=mean[:, :], in0=ps1[:, :], scalar1=1.0 / CW, scalar2=None, op0=ALU.mult), sig=s_d)
            k.wait("dve", s_d, d1)
            d2 = k.op("dve", lambda e: e.tensor_tensor(out=msq[:, :], in0=mean[:, :], in1=mean[:, :], op=ALU.mult), sig=s_d)
            k.wait("dve", s_d, d2)
            d3 = k.op("dve", lambda e, ps2=ps2: e.scalar_tensor_tensor(out=msq[:, :], in0=ps2[:, :], scalar=1.0 / CW, in1=msq[:, :], op0=ALU.mult, op1=ALU.subtract), sig=s_d)
            if tc == 0:
                dfree = {}
            dfree[tc] = d3
            k.wait("act", s_d, d3)
            a1 = k.op("act", lambda e: e.activation(out=rstd[:, :], in_=msq[:, :], func=AF.Ln, scale=1.0, bias=epsc[:, 0:1]), sig=s_a)
            k.wait("act", s_a, a1)
            a2 = k.op("act", lambda e: e.activation(out=rstd[:, :], in_=rstd[:, :], func=AF.Exp, scale=-0.5), sig=s_a)
            k.wait("dve", s_a, a2)
            for g in range(NG):
                yi = r_y.next("dve")
                e1 = k.op("dve", lambda e, g=g, yi=yi, cs=cs: e.tensor_tensor(out=yt[yi][:, :], in0=v[:, g, cs], in1=mean[:, :], op=ALU.subtract), sig=s_d)
                k.wait("dve", s_d, e1)
                e2 = k.op("dve", lambda e, yi=yi: e.tensor_tensor(out=yt[yi][:, :], in0=yt[yi][:, :], in1=rstd[:, :], op=ALU.mult), sig=s_d)
                k.wait("act", s_d, e2)
                r_y.release(yi, "act", lambda e, g=g, yi=yi, cs=cs: e.activation(out=conv_outT[:, g, cs], in_=yt[yi][:, :], func=AF.Silu, scale=lg[:, g:g + 1], bias=lb[:, g:g + 1]))
        for eng in ENGS:
            k.wait(eng, r_y.rel)
        k.end_stage()


def stage_attn(c):
    k, nc, psb = c["k"], c["nc"], c["psb"]
    kTd, vd, qTd, metat, ident, tri = c["kTd"], c["vd"], c["qTd"], c["metat"], c["ident"], c["tri"]
    print("sbuf before attn", nc.sbuf_bytes_remaining)
    onesb, identb = c["onesb"], c["identb"]
    attn_outT = c["attn_outT"]
    with contextlib.ExitStack() as st:
        sbt = lambda n, s, d: st.enter_context(nc.sbuf_tensor(n, list(s), d))
        cumT = sbt("a_cum", [8, TV], F32)
        lT = sbt("a_lT", [8, TV], F32)
        cumtok = sbt("a_ctok", [128, NT, NH], F32)
        refbc = sbt("a_ref", [128, NTO, NH], F32)
        tmpb = sbt("a_tmpb", [128, NT], F32)
        biasall = sbt("a_bias", [128, NH, NT, NTO], F32)
        maskb = sbt("a_mask", [128, 128], BF16)
        kT = [sbt(f"a_k{i}", [128, TV], BF16) for i in range(2)]
        vh = [sbt(f"a_v{i}", [128, NT, DH], BF16) for i in range(2)]
        qT = [sbt(f"a_q{i}", [128, TO], BF16) for i in range(2)]
        NP = 4
        pt = [sbt(f"a_p{i}", [128, 128], BF16) for i in range(NP)]
        rz = [sbt(f"a_rz{i}", [128, 128], F32) for i in range(2)]
        s_i = k.sem("a_i")
        s_v = k.sem("a_v")
        s_p = k.sem("a_pe0")
        k.dma("sync", lT[:, :], c["lTd"], s_i)
        k.wait("dve", s_i)
        v1 = k.op("dve", lambda e: e.tensor_tensor_scan(out=cumT[:, :], data0=lT[:, :], data1=lT[:, :], initial=0.0, op0=ALU.add, op1=ALU.max), sig=s_v)
        v2 = k.op("dve", lambda e: e.tensor_scalar(out=maskb[:, :], in0=tri, scalar1=-1.0, scalar2=-NEGB, op0=ALU.add, op1=ALU.mult), sig=s_v)
        k.wait("pe", s_v, v2)
        for kb in range(NT):
            k.op("pe", lambda e, kb=kb: e.transpose(psb[0][:, kb * 8:(kb + 1) * 8], cumT[0:8, kb * 128:(kb + 1) * 128], ident[0:8, 0:8]),
                 sig=(s_p if kb == NT - 1 else None))
        k.wait("dve", s_p)
        v3 = k.op("dve", lambda e: e.tensor_copy(out=cumtok[:].rearrange("p a b -> p (a b)"), in_=psb[0][:, 0:NT * NH]), sig=s_v)
        k.wait("pe", s_v, v3)
        pr = k.op("pe", lambda e: e.matmul(psb[1][:, 0:NTO * NH], lhsT=c["e64"], rhs=cumtok[:, NTO:NT, :].rearrange("p a b -> p (a b)"), start=True, stop=True), sig=s_p)
        k.op("pe", lambda e: e.matmul(psb[7][0:1, 0:2], lhsT=onesb[:, 0:1], rhs=onesb[:, 0:2], start=True, stop=True))
        k.wait("dve", s_p, pr)
        v4 = k.op("dve", lambda e: e.tensor_copy(out=refbc[:].rearrange("p a b -> p (a b)"), in_=psb[1][:, 0:NTO * NH]), sig=s_v)
        k.wait("dve", s_v, v4)
        for h in range(NH):
            v5 = k.op("dve", lambda e, h=h: e.tensor_tensor(out=tmpb[:, :], in0=cumtok[:, :, h], in1=metat[:, 0:NT], op=ALU.add), sig=s_v)
            k.wait("dve", s_v, v5)
            v6 = k.op("dve", lambda e, h=h: e.tensor_tensor(out=biasall[:, h, :, :], in0=tmpb[:, :].unsqueeze(2).to_broadcast([128, NT, NTO]),
                                                          in1=refbc[:, :, h].unsqueeze(1).to_broadcast([128, NT, NTO]), op=ALU.subtract), sig=s_v)
            k.wait("dve", s_v, v6)
        k.wait("act", s_v, v6)
        k.wait("pe", s_v, v6)

        s_kv = [k.sem("a_kv0"), k.sem("a_kv1")]
        r_kv = Ring(k, "a_rkv", 2)
        r_S = Ring(k, "a_rS", 5)
        r_P = Ring(k, "a_rP", NP)
        r_O = Ring(k, "a_rO", 2)
        s_S = k.sem("a_S")
        s_E = k.sem("a_E")
        s_O = k.sem("a_O")
        s_dz = k.sem("a_dz")

        def sslot(i):
            return psb[2 + i][:, 0:128]

        LOOK = globals().get('LOOK_RUN', 2)
        for h in range(globals().get('NH_RUN', NH)):
            hb = r_kv.next("sync")
            k.dma("sync", kT[hb][:], kTd[h, :, :], s_kv[hb])
            for q in range(4):
                k.dma("sync", vh[hb][:, 8 * q:8 * (q + 1), :], vd[8 * q:8 * (q + 1), :, h * DH:(h + 1) * DH].rearrange("t p n -> p t n"), s_kv[hb])
            kvv = k.dma("sync", qT[hb][:], qTd[h, :, :], s_kv[hb])
            k.wait("pe", s_kv[hb], kvv)
            seq = [(j, kb) for j in range(globals().get('NTO_RUN', NTO)) for kb in range(NTO + 1 + j)]
            state = {}
            inflight = []

            def emit_S(j, kb, h=h, hb=hb):
                si = r_S.next("pe")
                diag = (kb == NTO + j)
                sv = k.op("pe", lambda e: e.matmul(sslot(si), lhsT=kT[hb][:, kb * 128:(kb + 1) * 128], rhs=qT[hb][:, j * 128:(j + 1) * 128], start=True, stop=not diag),
                          sig=(None if diag else s_S))
                if diag:
                    sv = k.op("pe", lambda e: e.matmul(sslot(si), lhsT=identb[:, :], rhs=maskb[:, :], start=False, stop=True), sig=s_S)
                pi = r_P.next("act")
                k.wait("act", s_S, sv)
                ev = r_S.release(si, "act", lambda e: e.activation(out=pt[pi][:, :], in_=sslot(si), func=AF.Exp, bias=biasall[:, h, kb, j:j + 1], scale=1.0))
                return (j, kb, pi, ev)

            def emit_PV(j, kb, pi, ev, h=h, hb=hb):
                nk = NTO + 1 + j
                if kb == 0:
                    ob = r_O.next("pe")
                    state["ob"] = ob
                ob = state["ob"]
                pO = psb[ob][:, 0:128]
                pZ = psb[ob][:, 128:256]
                k.wait("pe", r_S.rel, ev)
                k.op("pe", lambda e: e.matmul(pO, lhsT=vh[hb][:, kb, :], rhs=pt[pi][:, :], start=(kb == 0), stop=(kb == nk - 1)))
                last = (kb == nk - 1)
                if not last:
                    r_P.release(pi, "pe", lambda e: e.matmul(pZ, lhsT=onesb[:, :], rhs=pt[pi][:, :], start=(kb == 0), stop=False))
                else:
                    ov = r_P.release(pi, "pe", lambda e: e.matmul(pZ, lhsT=onesb[:, :], rhs=pt[pi][:, :], start=(kb == 0), stop=True))
                    zi = j % 2
                    k.wait("dve", r_P.rel, ov)
                    z1 = k.op("dve", lambda e: e.reciprocal(out=rz[zi][:, :], in_=pZ), sig=s_dz)
                    k.wait("dve", s_dz, z1)
                    r_O.release(ob, "dve", lambda e: e.tensor_tensor(out=attn_outT[:, h, j * 128:(j + 1) * 128], in0=pO, in1=rz[zi][:, :], op=ALU.mult))

            for idx, (j, kb) in enumerate(seq):
                inflight.append(emit_S(j, kb))
                if len(inflight) > LOOK:
                    emit_PV(*inflight.pop(0))
            while inflight:
                emit_PV(*inflight.pop(0))
            r_kv.release(hb, "pe", lambda e: e.matmul(psb[7][0:1, 0:2], lhsT=onesb[:, 0:1], rhs=onesb[:, 0:2], start=True, stop=True))
        for eng in ENGS:
            k.wait(eng, r_O.rel)
            k.wait(eng, r_kv.rel)
        k.end_stage()


def chain(k, eng, sem, fns):
    v = None
    for fn in fns:
        if v is not None:
            k.wait(eng, sem, v)
        v = k.op(eng, fn, sig=sem)
    return v


def stage_outproj(c):
    print("sbuf before outproj", c["nc"].sbuf_bytes_remaining)
    k, nc, psb = c["k"], c["nc"], c["psb"]
    xv, w_out, x1d, modbc = c["xv"], c["w_out"], c["x1d"], c["modbc"]
    attn_outT, conv_outT = c["attn_outT"], c["conv_outT"]
    with contextlib.ExitStack() as st:
        sbt = lambda n, s, d: st.enter_context(nc.sbuf_tensor(n, list(s), d))
        wst = [sbt(f"o_ws{i}", [128, KD, 256], F32) for i in range(2)]
        wob = [sbt(f"o_wb{i}", [128, KD, 512], BF16) for i in range(2)]
        xp = [sbt(f"o_xp{i}", [128, 512], F32) for i in range(4)]
        tp = [sbt(f"o_tp{i}", [128, 512], F32) for i in range(2)]
        op_ = [sbt(f"o_op{i}", [128, 512], F32) for i in range(3)]
        s_ws = [k.sem("o_ws0"), k.sem("o_ws1")]
        r_ws = Ring(k, "o_rws", 2)
        r_wb = Ring(k, "o_rwb", 2)
        s_cast = k.sem("o_cast")
        s_x = [k.sem(f"o_x{i}") for i in range(4)]
        r_x = Ring(k, "o_rx", 4)
        r_ps = Ring(k, "o_rps", 4)
        r_o = Ring(k, "o_ro", 3)
        s_pe = k.sem("o_pe")
        s_d = k.sem("o_d")
        onesb = c["onesb"]
        for dg in range(4):
            wb = r_wb.next("dve")
            for half in range(2):
                ws = r_ws.next("sync")
                wv = k.dma("sync", wst[ws][:], w_out[:, dg * 512 + half * 256: dg * 512 + (half + 1) * 256].rearrange("(kk p) n -> p kk n", p=128), s_ws[ws])
                k.wait("dve", s_ws[ws], wv)
                cv = r_ws.release(ws, "dve", lambda e, ws=ws, wb=wb, half=half: e.tensor_copy(out=wob[wb][:, :, half * 256:(half + 1) * 256], in_=wst[ws][:, :, :]))
            k.wait("pe", r_ws.rel, cv)
            for i in range(NTO):
                xi = r_x.next("sync")
                xvv = k.dma("sync", xp[xi][:], xv[TO + i * 128:TO + (i + 1) * 128, dg * 512:(dg + 1) * 512], s_x[xi])
                bank = r_ps.next("pe")
                ps = psb[bank]
                for ch in range(16):
                    src = attn_outT if ch < 8 else conv_outT
                    k.op("pe", lambda e, ch=ch, src=src, ps=ps, i=i, wb=wb: e.matmul(ps[:, :], lhsT=src[:, ch % 8, i * 128:(i + 1) * 128], rhs=wob[wb][:, ch, :], start=(ch == 0), stop=(ch == 15)),
                         sig=(s_pe if ch == 15 else None))
                pv = s_pe.n
                ti = (dg * NTO + i) % 2
                oi = r_o.next("dve")
                k.wait("dve", s_pe, pv)
                k.wait("dve", s_x[xi], xvv)
                d1 = r_ps.release(bank, "dve", lambda e, ps=ps, ti=ti, dg=dg: e.tensor_tensor(out=tp[ti][:, :], in0=ps[:, :], in1=modbc[:, 0, dg * 512:(dg + 1) * 512], op=ALU.mult))
                k.wait("dve", r_ps.rel, d1)
                d2 = r_x.release(xi, "dve", lambda e, ti=ti, xi=xi, oi=oi: e.tensor_tensor(out=op_[oi][:, :], in0=tp[ti][:, :], in1=xp[xi][:, :], op=ALU.add))
                k.wait("sync", r_x.rel, d2)
                r_o.release_dma(oi, "sync", x1d[i * 128:(i + 1) * 128, dg * 512:(dg + 1) * 512], op_[oi][:, :])
            r_wb.release(wb, "pe", lambda e: e.matmul(psb[7][0:1, 0:2], lhsT=onesb[:, 0:1], rhs=onesb[:, 0:2], start=True, stop=True))
        for eng in ENGS:
            r_o.wait_all(eng)
            k.wait(eng, r_wb.rel)
        k.end_stage()


def stage_route(c):
    print("sbuf before route", c["nc"].sbuf_bytes_remaining)
    k, nc, psb = c["k"], c["nc"], c["psb"]
    x1d, modbc, w_r, b_r, xs, cntd = c["x1d"], c["modbc"], c["w_r"], c["b_r"], c["xs"], c["cntd"]
    ident, ones, ustr, ebase = c["ident"], c["ones"], c["ustr"], c["ebase"]
    w4, slot4i, WdT = c["w4"], c["slot4i"], c["WdT"]
    with contextlib.ExitStack() as st:
        sbt = lambda n, s, d: st.enter_context(nc.sbuf_tensor(n, list(s), d))
        x1 = [sbt(f"r_x{i}", [128, D], F32) for i in range(2)]
        h2 = sbt("r_h2", [128, D], F32)
        h2b = [sbt(f"r_hb{i}", [128, D], BF16) for i in range(2)]
        h2T = sbt("r_h2T", [128, KD, 128], F32)
        wr = sbt("r_wr", [128, KD, NE], F32)
        brb = sbt("r_br", [128, NE], F32)
        ssq = sbt("r_ssq", [128, NTO], F32)
        rs = sbt("r_rs", [128, NTO], F32)
        lg = sbt("r_lg", [128, NE], F32)
        mx = sbt("r_mx", [128, 8], F32)
        nm = sbt("r_nm", [128, 1], F32)
        e4 = sbt("r_e4", [128, 4], F32)
        es = sbt("r_es", [128, 1], F32)
        mask = sbt("r_mask", [128, NE], F32)
        carry = sbt("r_carry", [128, NE], F32)
        slotd = sbt("r_slotd", [128, NE], F32)
        oh = sbt("r_oh", [128, NE], F32)
        tmp = sbt("r_tmp", [128, NE], F32)
        wd = sbt("r_wd", [128, NE], F32)
        s4f = sbt("r_s4f", [128, 4], F32)
        cnti = sbt("r_cnti", [1, NE], I32)
        s_i = k.sem("r_i")
        k.dma("sync", wr[:], w_r.rearrange("(kk p) n -> p kk n", p=128), s_i)
        k.dma("sync", brb[:], b_r.partition_broadcast(128), s_i)
        k.op("dve", lambda e: e.memset(carry[:], 0.0))
        for eng in ("pe", "dve"):
            k.wait(eng, s_i)
        s_x = [k.sem("r_x0"), k.sem("r_x1")]
        s_a = k.sem("r_a")
        s_d = k.sem("r_d")
        s_pe = k.sem("r_pe")
        s_sc = [k.sem("r_sc0"), k.sem("r_sc1")]
        s_hb = k.sem("r_hb")
        for i in range(NTO):
            b = i % 2
            if i >= 2:
                k.wait("sync", s_d, xfree[i - 2])
            xvv = k.dma("sync", x1[b][:], x1d[i * 128:(i + 1) * 128, :], s_x[b])
            k.wait("act", s_x[b], xvv)
            if i >= 1:
                k.wait("act", s_pe, h2free[i - 1])
            a1 = k.op("act", lambda e, b=b, i=i: e.activation(out=h2[:, :], in_=x1[b][:, :], func=AF.Square, accum_out=ssq[:, i:i + 1]), sig=s_a)
            k.wait("dve", s_a, a1)
            d1 = k.op("dve", lambda e, i=i: e.tensor_scalar(out=rs[:, i:i + 1], in0=ssq[:, i:i + 1], scalar1=1.0 / D, scalar2=EPS, op0=ALU.mult, op1=ALU.add), sig=s_d)
            k.wait("act", s_d, d1)
            a2 = k.op("act", lambda e, i=i: e.activation(out=rs[:, i:i + 1], in_=rs[:, i:i + 1], func=AF.Sqrt), sig=s_a)
            k.wait("dve", s_a, a2)
            d4 = chain(k, "dve", s_d, [
                lambda e, i=i: e.reciprocal(out=rs[:, i:i + 1], in_=rs[:, i:i + 1]),
                lambda e, i=i, b=b: e.scalar_tensor_tensor(out=h2[:, :], in0=x1[b][:, :], scalar=rs[:, i:i + 1], in1=modbc[:, 1, :], op0=ALU.mult, op1=ALU.mult),
                lambda e: e.tensor_tensor(out=h2[:, :], in0=h2[:, :], in1=modbc[:, 2, :], op=ALU.add),
            ])
            if i == 0:
                xfree, h2free = {}, {}
            xfree[i] = d4
            k.wait("act", s_d, d4)
            if i >= 2:
                k.wait("act", s_sc[b], 64 * (i // 2))
            hb = k.op("act", lambda e, b=b: e.activation(out=h2b[b][:, :], in_=h2[:, :], func=AF.Copy), sig=s_hb)
            k.wait("pe", s_d, d4)
            for q4 in range(4):
                for kk4 in range(4):
                    kk = 4 * q4 + kk4
                    k.op("pe", lambda e, q4=q4, kk4=kk4, kk=kk: e.transpose(psb[q4][:, kk4 * 128:(kk4 + 1) * 128], h2[:, kk * 128:(kk + 1) * 128], ident),
                         sig=(s_pe if kk == 15 else None))
            tv = s_pe.n
            h2free[i] = tv
            k.wait("act", s_pe, tv)
            for q4 in range(4):
                av = k.op("act", lambda e, q4=q4: e.activation(out=h2T[:, 4 * q4:4 * q4 + 4, :].rearrange("p a b -> p (a b)"), in_=psb[q4][:, :], func=AF.Copy), sig=(s_a if q4 == 3 else None))
            k.wait("pe", s_a, av)
            for kk in range(KD):
                k.op("pe", lambda e, kk=kk: e.matmul(psb[4][:, 0:NE], lhsT=h2T[:, kk, :], rhs=wr[:, kk, :], start=(kk == 0), stop=(kk == KD - 1)), sig=(s_pe if kk == KD - 1 else None))
            lv = s_pe.n
            k.wait("dve", s_pe, lv)
            dm = chain(k, "dve", s_d, [
                lambda e: e.tensor_tensor(out=lg[:, :], in0=psb[4][:, 0:NE], in1=brb[:, :], op=ALU.add),
                lambda e: e.max(out=mx[:, :], in_=lg[:, :]),
                lambda e: e.tensor_scalar(out=mask[:, :], in0=lg[:, :], scalar1=mx[:, 3:4], scalar2=None, op0=ALU.is_ge),
                lambda e: e.tensor_scalar(out=nm[:, :], in0=mx[:, 0:1], scalar1=-1.0, scalar2=None, op0=ALU.mult),
            ])
            k.wait("pe", s_d, dm)
            k.op("pe", lambda e: e.matmul(psb[5][:, 0:NE], lhsT=ustr, rhs=mask[:, :], start=True, stop=True))
            rv = k.op("pe", lambda e: e.matmul(psb[5][:, NE:2 * NE], lhsT=ones, rhs=mask[:, :], start=True, stop=True), sig=s_pe)
            k.wait("act", s_d, dm)
            ev = k.op("act", lambda e: e.activation(out=e4[:, :], in_=mx[:, 0:4], func=AF.Exp, bias=nm[:, 0:1], scale=1.0, accum_out=es[:, 0:1]), sig=s_a)
            k.wait("dve", s_a, ev)
            k.wait("dve", s_pe, rv)
            fns = [
                lambda e: e.reciprocal(out=es[:, :], in_=es[:, :]),
                lambda e, i=i: e.tensor_scalar(out=w4[:, i, :], in0=e4[:, :], scalar1=es[:, 0:1], scalar2=None, op0=ALU.mult),
                lambda e: e.tensor_tensor(out=slotd[:, :], in0=psb[5][:, 0:NE], in1=carry[:, :], op=ALU.add),
                lambda e: e.tensor_scalar(out=slotd[:, :], in0=slotd[:, :], scalar1=float(CSB - 1), scalar2=None, op0=ALU.min),
                lambda e: e.tensor_tensor(out=slotd[:, :], in0=slotd[:, :], in1=ebase, op=ALU.add),
                lambda e: e.tensor_tensor(out=carry[:, :], in0=carry[:, :], in1=psb[5][:, NE:2 * NE], op=ALU.add),
            ]
            for kq in range(4):
                fns += [
                    lambda e, kq=kq: e.tensor_scalar(out=oh[:, :], in0=lg[:, :], scalar1=mx[:, kq:kq + 1], scalar2=None, op0=ALU.is_equal),
                    lambda e: e.tensor_tensor(out=tmp[:, :], in0=oh[:, :], in1=slotd[:, :], op=ALU.mult),
                    lambda e, kq=kq: e.tensor_reduce(out=s4f[:, kq:kq + 1], in_=tmp[:, :], axis=AX.X, op=ALU.add),
                ]
                if kq == 0:
                    fns.append(lambda e, i=i: e.tensor_scalar(out=wd[:, :], in0=oh[:, :], scalar1=w4[:, i, 0:1], scalar2=None, op0=ALU.mult))
                else:
                    fns.append(lambda e, i=i, kq=kq: e.scalar_tensor_tensor(out=wd[:, :], in0=oh[:, :], scalar=w4[:, i, kq:kq + 1], in1=wd[:, :], op0=ALU.mult, op1=ALU.add))
            fns.append(lambda e, i=i: e.tensor_copy(out=slot4i[:, i, :], in_=s4f[:, :]))
            dz = chain(k, "dve", s_d, fns)
            k.wait("pe", s_d, dz)
            tw = k.op("pe", lambda e: e.transpose(psb[6][0:NE, 0:128], wd[:, :], ident), sig=s_pe)
            k.wait("dve", s_pe, tw)
            k.wait("dve", s_d, dz)
            dw = k.op("dve", lambda e, i=i: e.tensor_copy(out=WdT[:, i, :], in_=psb[6][0:NE, 0:128]), sig=s_d)
            k.wait("pool", s_d, dz)
            k.wait("pool", s_hb, hb)
            if i == 0:
                k.wait("pool", c["s_z"])
            for kq in range(4):
                k.op("pool", lambda e, b=b, i=i, kq=kq: e.indirect_dma_start(
                    out=xs, out_offset=bass.IndirectOffsetOnAxis(ap=slot4i[:, i, kq:kq + 1], axis=0),
                    in_=h2b[b][:, :], in_offset=None), sig=s_sc[b], inc=16)
        k.wait("dve", s_d, dw)
        cz = k.op("dve", lambda e: e.tensor_copy(out=cnti[:, :], in_=carry[0:1, :]), sig=s_d)
        k.wait("sync", s_d, cz)
        fin = k.sem("r_fin")
        k.dma("sync", cntd[:, :], cnti[:, :], fin)
        k.wait("sync", s_sc[0])
        k.wait("sync", s_sc[1])
        k.wait("sync", fin)
        k.op("sync", lambda e: e.dma_start(out=cntd[0:1, 0:1], in_=cntd[0:1, 0:1]), sig=fin, inc=16)
        for eng in ENGS:
            k.wait(eng, fin)
        k.end_stage()


USE_IF = False


class Guard:
    def __init__(self, k, c, engs, thr):
        self.k, self.c, self.engs, self.thr = k, c, engs, thr

    def __enter__(self):
        k = self.k
        self.saved = {e: k.q[e] for e in self.engs}
        for e in self.engs:
            k.q[e] = []
        self.sig0 = None
        k._rec = {e: [] for e in self.engs}
        return self

    def __exit__(self, *a):
        k, c, thr = self.k, self.c, self.thr
        for eng in self.engs:
            body = k.q[eng]
            k.q[eng] = self.saved[eng]
            if not body:
                continue
            sigs = k._rec[eng]
            if not USE_IF:
                k.q[eng].extend(body)
                continue
            dummy = c["dummy"][eng]

            def f(e, body=body, sigs=sigs, eng=eng, dummy=dummy, thr=thr):
                n = k.cur[eng]
                with e.If(n > thr):
                    for g in body:
                        g(e)
                if sigs:
                    with e.Else():
                        for (h, inc) in sigs:
                            dummy(e).then_inc(h, inc)
            k.q[eng].append(f)
        k._rec = None
        return False


def _kb_op_rec(self, eng, fn, sig=None, inc=1):
    if sig is not None and getattr(self, "_rec", None) is not None and eng in self._rec:
        self._rec[eng].append((sig.h, inc))
    return KB._op_orig(self, eng, fn, sig=sig, inc=inc)


KB._op_orig = KB.op
KB.op = _kb_op_rec
KB._rec = None
KB.cur = None


def stage_moe(c):
    print("sbuf before moe", c["nc"].sbuf_bytes_remaining)
    k, nc, psb = c["k"], c["nc"], c["psb"]
    xs, osd, cntd = c["xs"], c["osd"], c["cntd"]
    w_g, w_u, w_d, b_g, b_u = c["w_g"], c["w_u"], c["w_d"], c["b_g"], c["b_u"]
    identb, onesb = c["identb"], c["onesb"]
    with contextlib.ExitStack() as st:
        sbt = lambda n, s, d: st.enter_context(nc.sbuf_tensor(n, list(s), d))
        xst = [sbt(f"e_xs{i}", [128, 2, D], BF16) for i in range(2)]
        xbT = sbt("e_xbT", [128, KD, CSB], BF16)
        hidT = sbt("e_hidT", [128, KD, CSB], BF16)
        wgb = [sbt(f"e_wg{i}", [128, KD, 128], BF16) for i in range(3)]
        wub = [sbt(f"e_wu{i}", [128, KD, 128], BF16) for i in range(3)]
        wdb = [sbt(f"e_wd{i}", [128, KD, 512], BF16) for i in range(2)]
        bg = sbt("e_bg", [128, NE, KD], F32)
        bu = sbt("e_bu", [128, NE, KD], F32)
        gp = [sbt(f"e_gp{i}", [128, TS], F32) for i in range(2)]
        sg = [sbt(f"e_sg{i}", [128, TS], F32) for i in range(2)]
        u1 = [sbt(f"e_u1{i}", [128, TS], F32) for i in range(2)]
        ob = [sbt(f"e_ob{i}", [128, 512], F32) for i in range(4)]
        scr = sbt("e_scr", [128, 8], F32)
        s_i = k.sem("e_i")
        k.dma("sync", bg[:], b_g, s_i)
        k.dma("sync", bu[:], b_u, s_i)
        for eng in ("act", "dve"):
            k.wait(eng, s_i)
        psbb = [p.bitcast(BF16) if hasattr(p, "bitcast") else p for p in psb]

        c["dummy"] = {
            "pe": lambda e: e.matmul(psb[7][0:1, 0:2], lhsT=onesb[:, 0:1], rhs=onesb[:, 0:2], start=True, stop=True),
            "act": lambda e: e.activation(out=scr[0:1, 0:1], in_=scr[0:1, 1:2], func=AF.Copy),
            "dve": lambda e: e.tensor_copy(out=scr[0:1, 2:3], in_=scr[0:1, 3:4]),
            "sync": lambda e: e.dma_start(out=cntd[0:1, NE - 1:NE], in_=cntd[0:1, NE - 1:NE]),
        }
        k.op("dve", lambda e: e.memset(scr[:], 0.0))
        k.cur = {}
        regs = {}

        def load_count(eng, ei):
            def f(e, eng=eng, ei=ei):
                if eng not in regs:
                    regs[eng] = e.alloc_register(f"cnt_{eng}")
                e.reg_load(regs[eng], cntd[0:1, ei:ei + 1])
                k.cur[eng] = e.snap(regs[eng])
            k.q[eng].append(f)

        s_xs = [k.sem("e_xs0"), k.sem("e_xs1")]
        r_xs = Ring(k, "e_rxs", 2)
        r_tp = Ring(k, "e_rtp", 3)
        s_tp = k.sem("e_tp")
        s_xb = k.sem("e_xb")
        s_wg = [k.sem(f"e_wg{i}") for i in range(3)]
        s_wu = [k.sem(f"e_wu{i}") for i in range(3)]
        r_wg = Ring(k, "e_rwg", 3)
        r_wu = Ring(k, "e_rwu", 3)
        s_wd = [k.sem("e_wd0"), k.sem("e_wd1")]
        r_wd = Ring(k, "e_rwd", 2)
        r_gu = Ring(k, "e_rgu", 4)
        s_gu = k.sem("e_gu")
        s_gp = k.sem("e_gp")
        s_sg = k.sem("e_sg")
        s_u1 = k.sem("e_u1")
        s_hid = k.sem("e_hid")
        r_et = Ring(k, "e_ret", 2)
        r_dn = Ring(k, "e_rdn", 4)
        s_dn = k.sem("e_dn")
        r_ob = Ring(k, "e_rob", 4)
        s_ob = k.sem("e_ob")
        s_gud = k.sem("e_gud")
        s_dnd = k.sem("e_dnd")
        gud_val = {}
        dnd_val = {}
        hid_last = {}

        for ei in range(globals().get('NE_RUN', NE)):
            for eng in ("pe", "act", "dve", "sync"):
                load_count(eng, ei)
            if ei >= 1:
                k.wait("act", s_gud, gud_val[ei - 1])
            for j in range(NJ):
                with Guard(k, c, ("sync", "pe", "act"), TS * j):
                    xi = r_xs.next("sync")
                    base = ei * CSB + j * TS
                    xv_ = k.dma("sync", xst[xi][:], xs[base:base + TS, :].rearrange("(h p) d -> p h d", p=128), s_xs[xi])
                    k.wait("pe", s_xs[xi], xv_)
                    for q4 in range(4):
                        bank = r_tp.next("pe")
                        for kk4 in range(4):
                            kk = 4 * q4 + kk4
                            for hh in range(2):
                                last = (kk4 == 3 and hh == 1)
                                fn = lambda e, bank=bank, kk4=kk4, hh=hh, xi=xi, kk=kk: e.transpose(
                                    psbb[4 + bank][:, kk4 * 256 + hh * 128: kk4 * 256 + (hh + 1) * 128], xst[xi][:, hh, kk * 128:(kk + 1) * 128], identb[:, :])
                                if last and q4 == 3:
                                    tv = r_xs.release(xi, "pe", fn)
                                    k.wait("act", r_xs.rel, tv)
                                elif last:
                                    tv = k.op("pe", fn, sig=s_tp)
                                    k.wait("act", s_tp, tv)
                                else:
                                    k.op("pe", fn)
                        r_tp.release(bank, "act", lambda e, bank=bank, q4=q4, j=j: e.activation(
                            out=xbT[:, 4 * q4:4 * q4 + 4, j * TS:(j + 1) * TS], in_=psbb[4 + bank][:, 0:1024].rearrange("p (a b) -> p a b", b=TS), func=AF.Copy))
                    k.op("act", lambda e: e.activation(out=scr[0:1, 4:5], in_=scr[0:1, 5:6], func=AF.Copy), sig=s_xb)
            xb_ready = s_xb.n
            k.wait("pe", s_xb, xb_ready)
            if ei >= 1:
                k.wait("dve", s_dnd, dnd_val[ei - 1])
            k.wait("pe", r_dn.rel)
            for fc in range(KD):
                gi = r_wg.next("pool")
                gv = k.dma("pool", wgb[gi][:], w_g[ei, :, fc * 128:(fc + 1) * 128].rearrange("(kk p) n -> p kk n", p=128), s_wg[gi])
                ui = r_wu.next("pool")
                uv = k.dma("pool", wub[ui][:], w_u[ei, :, fc * 128:(fc + 1) * 128].rearrange("(kk p) n -> p kk n", p=128), s_wu[ui])
                k.wait("pe", s_wg[gi], gv)
                k.wait("pe", s_wu[ui], uv)
                for j in range(NJ):
                    with Guard(k, c, ("pe", "act", "dve"), TS * j):
                        bank = r_gu.next("pe")
                        ps = psb[bank]
                        cs = slice(j * TS, (j + 1) * TS)
                        for kk in range(KD):
                            k.op("pe", lambda e, kk=kk, ps=ps, gi=gi, cs=cs: e.matmul(ps[:, 0:TS], lhsT=wgb[gi][:, kk, :], rhs=xbT[:, kk, cs], start=(kk == 0), stop=(kk == KD - 1)))
                        for kk in range(KD):
                            k.op("pe", lambda e, kk=kk, ps=ps, ui=ui, cs=cs: e.matmul(ps[:, TS:2 * TS], lhsT=wub[ui][:, kk, :], rhs=xbT[:, kk, cs], start=(kk == 0), stop=(kk == KD - 1)),
                                 sig=(s_gu if kk == KD - 1 else None))
                        pv = s_gu.n
                        ti = r_et.next("dve", "act")
                        k.wait("dve", s_gu, pv)
                        k.wait("act", s_gu, pv)
                        d1 = k.op("dve", lambda e, ps=ps, ti=ti, ei=ei, fc=fc: e.tensor_scalar(out=gp[ti][:, :], in0=ps[:, 0:TS], scalar1=bg[:, ei, fc:fc + 1], scalar2=SWIGLU_LIMIT, op0=ALU.add, op1=ALU.min), sig=s_gp)
                        k.wait("act", s_gp, d1)
                        a1 = k.op("act", lambda e, ps=ps, ti=ti, ei=ei, fc=fc: e.activation(out=u1[ti][:, :], in_=ps[:, TS:2 * TS], func=AF.Identity, bias=bu[:, ei, fc:fc + 1], scale=1.0), sig=s_u1)
                        a2 = k.op("act", lambda e, ti=ti: e.activation(out=sg[ti][:, :], in_=gp[ti][:, :], func=AF.Sigmoid, scale=SWIGLU_ALPHA), sig=s_sg)
                        r_gu.release(bank, "act", lambda e: e.activation(out=scr[0:1, 6:7], in_=scr[0:1, 7:8], func=AF.Copy))
                        k.wait("dve", s_u1, a1)
                        d2 = k.op("dve", lambda e, ti=ti: e.tensor_scalar(out=u1[ti][:, :], in0=u1[ti][:, :], scalar1=-SWIGLU_LIMIT, scalar2=SWIGLU_LIMIT, op0=ALU.max, op1=ALU.min), sig=s_gp)
                        k.wait("dve", s_sg, a2)
                        d3 = k.op("dve", lambda e, ti=ti: e.tensor_tensor(out=gp[ti][:, :], in0=gp[ti][:, :], in1=sg[ti][:, :], op=ALU.mult), sig=s_gp)
                        k.wait("dve", s_gp, d3)
                        d4 = k.op("dve", lambda e, ti=ti, fc=fc, cs=cs: e.scalar_tensor_tensor(out=hidT[:, fc, cs], in0=u1[ti][:, :], scalar=1.0, in1=gp[ti][:, :], op0=ALU.add, op1=ALU.mult), sig=s_hid)
                        k.wait("dve", s_hid, d4)
                        r_et.release(ti, "dve", lambda e: e.tensor_copy(out=scr[0:1, 2:3], in_=scr[0:1, 3:4]))
                r_wg.release(gi, "pe", c["dummy"]["pe"])
                r_wu.release(ui, "pe", c["dummy"]["pe"])
            gud_val[ei] = k.op("pe", c["dummy"]["pe"], sig=s_gud)
            hid_ready = s_hid.n
            k.wait("pe", s_hid, hid_ready)
            k.wait("pe", r_gu.rel)
            for dg in range(4):
                wi = r_wd.next("pool")
                wv = k.dma("pool", wdb[wi][:], w_d[ei, :, dg * 512:(dg + 1) * 512].rearrange("(kk p) n -> p kk n", p=128), s_wd[wi])
                k.wait("pe", s_wd[wi], wv)
                for j in range(NJ):
                    with Guard(k, c, ("pe", "act", "sync"), TS * j):
                        for hh in range(2):
                            bank = r_dn.next("pe")
                            ps = psb[bank]
                            s0 = j * TS + hh * 128
                            for fc in range(KD):
                                k.op("pe", lambda e, fc=fc, ps=ps, wi=wi, s0=s0: e.matmul(ps[:, :], lhsT=hidT[:, fc, s0:s0 + 128], rhs=wdb[wi][:, fc, :], start=(fc == 0), stop=(fc == KD - 1)),
                                     sig=(s_dn if fc == KD - 1 else None))
                            pv = s_dn.n
                            oi = r_ob.next("act")
                            k.wait("act", s_dn, pv)
                            av = r_dn.release(bank, "act", lambda e, ps=ps, oi=oi: e.activation(out=ob[oi][:, :], in_=ps[:, :], func=AF.Copy))
                            k.wait("sync", r_dn.rel, av)
                            row = ei * CSB + s0
                            r_ob.release_dma(oi, "sync", osd[row:row + 128, dg * 512:(dg + 1) * 512], ob[oi][:, :])
                r_wd.release(wi, "pe", c["dummy"]["pe"])
            dnd_val[ei] = k.op("pe", c["dummy"]["pe"], sig=s_dnd)
        for eng in ENGS:
            r_ob.wait_all(eng)
            k.wait(eng, s_dnd)
        k.end_stage()


SWIGLU_LIMIT = 7.0
SWIGLU_ALPHA = 1.702


def stage_combine(c):
    print("sbuf before combine", c["nc"].sbuf_bytes_remaining)
    k, nc, psb = c["k"], c["nc"], c["psb"]
    x1d, osd, out, b_d = c["x1d"], c["osd"], c["out"], c["b_d"]
    w4, slot4i, WdT, gate2 = c["w4"], c["slot4i"], c["WdT"], c["gate2"]
    with contextlib.ExitStack() as st:
        sbt = lambda n, s, d: st.enter_context(nc.sbuf_tensor(n, list(s), d))
        x1 = [sbt(f"f_x{i}", [128, D], F32) for i in range(2)]
        gb = [[sbt(f"f_g{b}{q}", [128, D], F32) for q in range(4)] for b in range(2)]
        acc = [sbt(f"f_a{i}", [128, D], F32) for i in range(2)]
        bd = sbt("f_bd", [NE, D], F32)
        s_i = k.sem("f_i")
        k.dma("sync", bd[:], b_d, s_i)
        k.wait("pe", s_i)
        s_x = [k.sem("f_x0"), k.sem("f_x1")]
        s_g = [k.sem("f_g0"), k.sem("f_g1")]
        r_g = Ring(k, "f_rg", 2)
        r_x = Ring(k, "f_rx", 2)
        r_ps = Ring(k, "f_rps", 2)
        r_a = Ring(k, "f_ra", 2)
        s_pe = k.sem("f_pe")
        s_d = k.sem("f_d")
        for i in range(NTO):
            xb = r_x.next("sync")
            xvv = k.dma("sync", x1[xb][:], x1d[i * 128:(i + 1) * 128, :], s_x[xb])
            gbi = r_g.next("pool")
            for kq in range(4):
                k.op("pool", lambda e, gbi=gbi, kq=kq, i=i: e.indirect_dma_start(
                    out=gb[gbi][kq][:, :], out_offset=None, in_=osd,
                    in_offset=bass.IndirectOffsetOnAxis(ap=slot4i[:, i, kq:kq + 1], axis=0)), sig=s_g[gbi], inc=16)
            gv = s_g[gbi].n
            pset = r_ps.next("pe")
            for dg in range(4):
                k.op("pe", lambda e, pset=pset, dg=dg, i=i: e.matmul(psb[4 * pset + dg][:, :], lhsT=WdT[:, i, :], rhs=bd[:, dg * 512:(dg + 1) * 512], start=True, stop=True),
                     sig=(s_pe if dg == 3 else None))
            pv = s_pe.n
            ai = r_a.next("dve")
            k.wait("dve", s_g[gbi], gv)
            fns = [lambda e, ai=ai, gbi=gbi, i=i: e.tensor_scalar(out=acc[ai][:, :], in0=gb[gbi][0][:, :], scalar1=w4[:, i, 0:1], scalar2=None, op0=ALU.mult)]
            for kq in range(1, 4):
                fns.append(lambda e, ai=ai, gbi=gbi, i=i, kq=kq: e.scalar_tensor_tensor(out=acc[ai][:, :], in0=gb[gbi][kq][:, :], scalar=w4[:, i, kq:kq + 1], in1=acc[ai][:, :], op0=ALU.mult, op1=ALU.add))
            d1 = chain(k, "dve", s_d, fns)
            k.wait("dve", s_d, d1)
            r_g.release(gbi, "dve", lambda e, ai=ai: e.tensor_copy(out=acc[ai][0:1, 0:1], in_=acc[ai][0:1, 0:1]))
            k.wait("dve", s_pe, pv)
            k.wait("dve", r_g.rel)
            fns = [lambda e, ai=ai, pset=pset, dg=dg: e.tensor_tensor(out=acc[ai][:, dg * 512:(dg + 1) * 512], in0=acc[ai][:, dg * 512:(dg + 1) * 512], in1=psb[4 * pset + dg][:, :], op=ALU.add) for dg in range(4)]
            d2 = chain(k, "dve", s_d, fns)
            k.wait("dve", s_d, d2)
            r_ps.release(pset, "dve", lambda e, ai=ai: e.tensor_copy(out=acc[ai][0:1, 0:1], in_=acc[ai][0:1, 0:1]))
            k.wait("dve", r_ps.rel)
            k.wait("dve", s_x[xb], xvv)
            d3 = chain(k, "dve", s_d, [
                lambda e, ai=ai: e.tensor_tensor(out=acc[ai][:, :], in0=acc[ai][:, :], in1=gate2[:, :], op=ALU.mult),
            ])
            k.wait("dve", s_d, d3)
            d4 = r_x.release(xb, "dve", lambda e, ai=ai, xb=xb: e.tensor_tensor(out=acc[ai][:, :], in0=acc[ai][:, :], in1=x1[xb][:, :], op=ALU.add))
            k.wait("sync", r_x.rel, d4)
            r_a.release_dma(ai, "sync", out[i * 128:(i + 1) * 128, :], acc[ai][:, :])
        for eng in ENGS:
            r_a.wait_all(eng)
        k.end_stage()
```

```python
import contextlib
import numpy as np
import ml_dtypes
import concourse.bass as bass
import concourse.mybir as mybir
from concourse.bass_utils import run_bass_kernel_spmd

F32 = mybir.dt.float32
F32R = mybir.dt.float32r
BF16 = mybir.dt.bfloat16
I32 = mybir.dt.int32
AF = mybir.ActivationFunctionType
ALU = mybir.AluOpType
AX = mybir.AxisListType

D = 2048
KD = 16
TV = 4096
NT = 32
TO = 2048
NTO = 16
NH = 8
DH = 128
CW = 1024
NG = 8
CK = 31
INC = 5128
NE = 32
CSB = 1024
TS = 256
NJ = CSB // TS
EPS = 1e-6
HALO = 256
NEGB = -30000.0

ENGS = ("sync", "act", "pool", "pe", "dve")


class Sem:
    def __init__(self, h):
        self.h = h
        self.n = 0


class KB:
    def __init__(self, nc, stack):
        self.nc = nc
        self.stack = stack
        self.q = {e: [] for e in ENGS}
        self.nsem = 0
        self.stage_sems = []

    def sem(self, name, persist=False):
        self.nsem += 1
        h = self.nc.alloc_semaphore(name=name)
        if not persist:
            self.stage_sems.append(h)
        return Sem(h)

    def end_stage(self):
        self.flush()
        if self.stage_sems:
            self.nc.all_engine_barrier()
            self.nc.clear_and_free_semaphores(self.stage_sems)
            self.nc.all_engine_barrier()
            self.stage_sems = []

    def sb(self, name, shape, dt):
        return self.stack.enter_context(self.nc.sbuf_tensor(name, list(shape), dt))

    def op(self, eng, fn, sig=None, inc=1):
        if sig is not None:
            sig.n += inc
            h = sig.h

            def f(e, fn=fn, h=h, inc=inc):
                fn(e).then_inc(h, inc)
            self.q[eng].append(f)
            return sig.n
        self.q[eng].append(lambda e, fn=fn: fn(e))
        return None

    def dma(self, eng, out, in_, sig, **kw):
        return self.op(eng, lambda e: e.dma_start(out=out, in_=in_, **kw), sig=sig, inc=16)

    def wait(self, eng, sem, val=None):
        v = sem.n if val is None else val
        if v <= 0:
            return
        self.q[eng].append(lambda e, h=sem.h, v=v: e.wait_ge(h, v))

    def raw(self, eng, fn):
        self.q[eng].append(fn)

    def flush(self):
        q = self.q
        self.q = {e: [] for e in ENGS}
        with self.nc.Block() as blk:
            @blk.sync
            def _(e):
                for f in q["sync"]:
                    f(e)

            @blk.scalar
            def _(e):
                for f in q["act"]:
                    f(e)

            @blk.gpsimd
            def _(e):
                for f in q["pool"]:
                    f(e)

            @blk.tensor
            def _(e):
                for f in q["pe"]:
                    f(e)

            @blk.vector
            def _(e):
                for f in q["dve"]:
                    f(e)


def build(stage="full", dbg_shape=None):
    nc = bass.Bass("TRN2", target_bir_lowering=False)

    def din(name, shape, dt=F32):
        return nc.dram_tensor(name, list(shape), dt, kind="ExternalInput").ap()

    def dscr(name, shape, dt=F32):
        return nc.dram_tensor(name, list(shape), dt, kind="Internal").ap()

    xv = din("xv", [TV, D])
    meta = din("meta", [128, 64])
    cT = din("cT", [128, KD])
    ada_w = din("ada_w", [D, 6 * D])
    ada_b = din("ada_b", [1, 6 * D])
    g1T = din("g1T", [128, KD])
    g2 = din("g2", [1, D])
    w_in = din("w_in", [D, INC])
    bfc = din("bfc", [8, 1])
    gq = din("gq", [128, 1])
    gk = din("gk", [128, 1])
    cwT = din("cwT", [128, NG, CK])
    cbT = din("cbT", [128, NG])
    lgT = din("lgT", [128, NG])
    lbT = din("lbT", [128, NG])
    w_out = din("w_out", [D, D])
    w_r = din("w_r", [D, NE])
    b_r = din("b_r", [1, NE])
    consts = din("consts", [128, 1024])
    sel8 = din("sel8", [8, 8 * 128])
    if stage in ("full", "moe"):
        w_g = din("w_g", [NE, D, D])
        b_g = din("b_g", [128, NE, KD])
        w_u = din("w_u", [NE, D, D])
        b_u = din("b_u", [128, NE, KD])
        w_d = din("w_d", [NE, D, D])
        b_d = din("b_d", [NE, D])
    out = nc.dram_tensor("out", [TO, D], F32, kind="ExternalOutput").ap()
    dbg = None
    if dbg_shape is not None:
        dbg = nc.dram_tensor("dbg", list(dbg_shape), F32, kind="ExternalOutput").ap()

    hTd = dscr("hTd", [KD, 128, TV])
    kTd = dscr("kTd", [NH, 128, TV], BF16)
    vd = dscr("vd", [NT, 128, NH * DH], BF16)
    qTd = dscr("qTd", [NH, 128, TO], BF16)
    u0d = dscr("u0d", [NG, 128, HALO + TO])
    x1d = dscr("x1d", [TO, D])
    xs = dscr("xs", [NE * CSB, D], BF16)
    osd = dscr("osd", [NE * CSB, D])
    cntd = dscr("cntd", [1, NE], I32)
    lTd = dscr("lTd", [8, TV])

    with contextlib.ExitStack() as stack:
        k = KB(nc, stack)
        cst = k.sb("cst", [128, 1024], F32)
        ident = cst[:, 0:128]
        tri = cst[:, 128:256]
        ustr = cst[:, 256:384]
        ones = cst[:, 384:512]
        ebase = cst[:, 512:544]
        e64 = cst[:, 544:672]
        metat = k.sb("metat", [128, 64], F32)
        identb = k.sb("identb", [128, 128], BF16)
        onesb = k.sb("onesb", [128, 128], BF16)
        gate2 = k.sb("gate2", [128, D], F32)
        gqc = k.sb("gqc", [128, 1], F32)
        gkc = k.sb("gkc", [128, 1], F32)
        w4 = k.sb("w4", [128, NTO, 4], F32)
        slot4i = k.sb("slot4i", [128, NTO, 4], I32)
        WdT = k.sb("WdT", [NE, NTO, 128], F32)
        psb = [stack.enter_context(nc.psum_tensor(f"ps{i}", [128, 512], F32)) for i in range(8)]

        s_ld = k.sem("s_ld", persist=True)
        s_c0 = k.sem("s_c0", persist=True)
        k.dma("sync", cst[:], consts, s_ld)
        k.dma("sync", metat[:], meta, s_ld)
        k.dma("sync", gqc[:], gq, s_ld)
        k.dma("sync", gkc[:], gk, s_ld)
        k.wait("dve", s_ld)
        k.op("dve", lambda e: e.tensor_copy(out=identb[:], in_=ident))
        k.op("dve", lambda e: e.tensor_scalar(out=gqc[:], in0=gqc[:], scalar1=float(DH ** -0.5), scalar2=None, op0=ALU.mult))
        k.op("dve", lambda e: e.tensor_copy(out=onesb[:], in_=ones), sig=s_c0)

        ctx = dict(locals())
        ctx["k"] = k
        ctx["stack"] = stack
        with contextlib.ExitStack() as sA:
            ctx["modbc"] = sA.enter_context(nc.sbuf_tensor("modbc", [128, 3, D], F32))
            ctx["a1c"] = sA.enter_context(nc.sbuf_tensor("a1c", [128, KD], F32))
            ctx["b1c"] = sA.enter_context(nc.sbuf_tensor("b1c", [128, KD], F32))
            stage_mod(ctx)
            stage_ht(ctx)
            if stage == "s1":
                finish_dbg(ctx, "s1")
                return nc
            with contextlib.ExitStack() as sB:
                stage_proj(ctx)
                if stage == "s2":
                    finish_dbg(ctx, "s2")
                    return nc
                with contextlib.ExitStack() as sC:
                    ctx["conv_outT"] = sC.enter_context(nc.sbuf_tensor("conv_outT", [128, NG, TO], BF16))
                    ctx["attn_outT"] = sC.enter_context(nc.sbuf_tensor("attn_outT", [128, NH, TO], BF16))
                    stage_conv(ctx)
                    if stage == "s3":
                        finish_dbg(ctx, "s3")
                        return nc
                    stage_attn(ctx)
                    if stage == "s4":
                        finish_dbg(ctx, "s4")
                        return nc
                    stage_outproj(ctx)
            stage_route(ctx)
            if stage == "s6":
                finish_dbg(ctx, "s6")
                return nc
        stage_moe(ctx)
        stage_combine(ctx)
    return nc


def stage_mod(c):
    k, nc, stack, psb = c["k"], c["nc"], c["stack"], c["psb"]
    cT, ada_w, ada_b, g1T, g2 = c["cT"], c["ada_w"], c["ada_b"], c["g1T"], c["g2"]
    modbc, a1c, b1c, ident, ones = c["modbc"], c["a1c"], c["b1c"], c["ident"], c["ones"]
    with contextlib.ExitStack() as st:
        ct = st.enter_context(nc.sbuf_tensor("m_ct", [128, KD], F32))
        sg = st.enter_context(nc.sbuf_tensor("m_sg", [128, KD], F32))
        cbc = st.enter_context(nc.sbuf_tensor("m_cbc", [128, KD, 128], F32))
        abt = st.enter_context(nc.sbuf_tensor("m_ab", [1, 6 * D], F32))
        g1t = st.enter_context(nc.sbuf_tensor("m_g1", [128, KD], F32))
        g2b = st.enter_context(nc.sbuf_tensor("m_g2b", [128, D], F32))
        wbuf = [st.enter_context(nc.sbuf_tensor(f"m_w{i}", [128, KD, 512], F32)) for i in range(2)]
        ss1 = st.enter_context(nc.sbuf_tensor("m_ss1", [128, 2, D], F32))
        tmp = st.enter_context(nc.sbuf_tensor("m_tmp", [128, KD, 128], F32))
        zt = st.enter_context(nc.sbuf_tensor("m_zt", [128, D], BF16))
        s_z0 = k.sem("m_z0")
        s_z = k.sem("zfill", persist=True)
        c["s_z"] = s_z
        k.op("dve", lambda e: e.memset(zt[:], 0.0), sig=s_z0)
        k.wait("pool", s_z0)
        xs_ = c["xs"]
        zv = k.dma("pool", xs_[0:128, :], zt[:], s_z)
        zfirst = zv
        nz = 128
        while nz < NE * CSB:
            k.wait("pool", s_z, zv)
            step = min(nz, 2048)
            for r0 in range(nz, min(2 * nz, NE * CSB), step):
                zv = k.dma("pool", xs_[r0:r0 + step, :], xs_[0:step, :], s_z)
            nz *= 2
        s_in = k.sem("m_in")
        s_w = [k.sem("m_w0"), k.sem("m_w1")]
        s_fr = k.sem("m_fr")
        s_pe = k.sem("m_pe")
        s_ev = k.sem("m_ev")
        s_a = k.sem("m_a")
        s_v = k.sem("m_v")
        k.dma("sync", ct[:], cT, s_in)
        k.dma("sync", abt[:], ada_b, s_in)
        k.dma("sync", g1t[:], g1T, s_in)
        k.dma("sync", g2b[:], g2.partition_broadcast(128), s_in)
        k.wait("act", s_in)
        k.op("act", lambda e: e.activation(out=sg[:], in_=ct[:], func=AF.Sigmoid), sig=s_a)
        k.wait("dve", s_a)
        k.wait("dve", s_in)
        k.op("dve", lambda e: e.tensor_mul(out=sg[:], in0=sg[:], in1=ct[:]), sig=s_v)
        k.wait("dve", s_v)
        vb = k.op("dve", lambda e: e.tensor_copy(out=cbc[:], in_=sg[:].unsqueeze(2).to_broadcast([128, KD, 128])), sig=s_v)
        k.wait("pe", s_v, vb)
        k.wait("pe", s_in)
        k.wait("pe", c["s_ld"])
        NGp = 24
        for gi in range(NGp):
            b = gi % 2
            if gi >= 2:
                k.wait("sync", s_fr, gi - 1)
            src = ada_w[:, gi * 512:(gi + 1) * 512].rearrange("(kk p) n -> p kk n", p=128)
            wv = k.dma("sync", wbuf[b][:], src, s_w[b])
            k.wait("pe", s_w[b], wv)
            ps = psb[gi % 4]
            if gi >= 4:
                k.wait("pe", s_ev, gi - 3)
            for kk in range(KD):
                k.op("pe", lambda e, ps=ps, kk=kk, b=b: e.matmul(ps[:, :], lhsT=cbc[:, kk, :], rhs=wbuf[b][:, kk, :], start=(kk == 0), stop=False),
                     sig=(s_fr if kk == KD - 1 else None))
            pv = k.op("pe", lambda e, ps=ps, gi=gi: e.matmul(ps[:, :], lhsT=ones[0:1, :], rhs=abt[0:1, gi * 512:(gi + 1) * 512], start=False, stop=True), sig=s_pe)
            k.wait("act", s_pe, pv)
            which, sub = gi // 4, gi % 4
            if which in (0, 1):
                dst = ss1[:, which, sub * 512:(sub + 1) * 512]
            elif which == 2:
                dst = modbc[:, 0, sub * 512:(sub + 1) * 512]
            elif which == 3:
                dst = modbc[:, 2, sub * 512:(sub + 1) * 512]
            elif which == 4:
                dst = modbc[:, 1, sub * 512:(sub + 1) * 512]
            else:
                dst = c["gate2"][:, sub * 512:(sub + 1) * 512]
            k.op("act", lambda e, dst=dst, ps=ps: e.activation(out=dst, in_=ps[:, :], func=AF.Copy), sig=s_ev)
        k.wait("dve", s_ev)
        s_d = k.sem("m_d")
        for which, dstc in ((0, b1c), (1, a1c)):
            v1 = k.op("dve", lambda e, which=which: e.tensor_tensor(
                out=tmp[:], in0=ss1[:, which, :].rearrange("p (a b) -> p a b", b=128),
                in1=ident.unsqueeze(1).to_broadcast([128, KD, 128]), op=ALU.mult), sig=s_d)
            k.wait("dve", s_d, v1)
            v2 = k.op("dve", lambda e, dstc=dstc: e.tensor_reduce(out=dstc[:], in_=tmp[:], axis=AX.X, op=ALU.add), sig=s_d)
            k.wait("dve", s_d, v2)
        v3 = k.op("dve", lambda e: e.scalar_tensor_tensor(out=a1c[:], in0=a1c[:], scalar=1.0, in1=g1t[:], op0=ALU.add, op1=ALU.mult), sig=s_d)
        v4 = k.op("dve", lambda e: e.scalar_tensor_tensor(out=modbc[:, 1, :], in0=modbc[:, 1, :], scalar=1.0, in1=g2b[:], op0=ALU.add, op1=ALU.mult), sig=s_d)
        c["s_mod"] = s_d
        for eng in ENGS:
            k.wait(eng, s_d)
        k.wait("pool", s_z, zfirst)
        k.end_stage()


def stage_ht(c):
    k, nc, psb = c["k"], c["nc"], c["psb"]
    xv, hTd, a1c, b1c, ident = c["xv"], c["hTd"], c["a1c"], c["b1c"], c["ident"]
    with contextlib.ExitStack() as st:
        xt = [st.enter_context(nc.sbuf_tensor(f"h_x{i}", [128, D], F32)) for i in range(2)]
        xn = [st.enter_context(nc.sbuf_tensor(f"h_xn{i}", [128, D], F32)) for i in range(2)]
        junk = st.enter_context(nc.sbuf_tensor("h_junk", [128, D], F32))
        ssq = st.enter_context(nc.sbuf_tensor("h_ssq", [128, NT], F32))
        rstd = st.enter_context(nc.sbuf_tensor("h_rstd", [128, NT], F32))
        hs = [st.enter_context(nc.sbuf_tensor(f"h_hs{i}", [128, KD, 512], F32)) for i in range(2)]
        s_x = [k.sem("h_x0"), k.sem("h_x1")]
        s_sq = k.sem("h_sq")
        s_rs = k.sem("h_rs")
        s_xn = k.sem("h_xn")
        s_tp = k.sem("h_tp")
        s_eva = k.sem("h_eva")
        s_evv = k.sem("h_evv")
        s_st = [k.sem("h_st0"), k.sem("h_st1")]
        for i in range(NT):
            b = i % 2
            if i >= 2:
                k.wait("sync", s_xn, i - 1)
            xvv = k.dma("sync", xt[b][:], xv[i * 128:(i + 1) * 128, :], s_x[b])
            k.wait("act", s_x[b], xvv)
            sv = k.op("act", lambda e, b=b, i=i: e.activation(out=junk[:], in_=xt[b][:], func=AF.Square, accum_out=ssq[:, i:i + 1]), sig=s_sq)
            k.wait("dve", s_sq, sv)
            r1 = k.op("dve", lambda e, i=i: e.tensor_scalar(out=rstd[:, i:i + 1], in0=ssq[:, i:i + 1], scalar1=1.0 / D, scalar2=EPS, op0=ALU.mult, op1=ALU.add), sig=s_rs)
            k.wait("act", s_rs, r1)
            rq = k.op("act", lambda e, i=i: e.activation(out=rstd[:, i:i + 1], in_=rstd[:, i:i + 1], func=AF.Sqrt), sig=s_sq)
            k.wait("dve", s_sq, rq)
            r2 = k.op("dve", lambda e, i=i: e.reciprocal(out=rstd[:, i:i + 1], in_=rstd[:, i:i + 1]), sig=s_rs)
            k.wait("act", s_rs, r2)
            if i >= 2:
                k.wait("act", s_tp, 4 * (i - 1))
            nv = k.op("act", lambda e, b=b, i=i: e.activation(out=xn[b][:], in_=xt[b][:], func=AF.Identity, scale=rstd[:, i:i + 1]), sig=s_xn)
            k.wait("pe", s_xn, nv)
            hb = (i // 4) % 2
            sub = i % 4
            ch = i // 4
            if sub == 0 and ch >= 2:
                k.wait("act", s_st[hb], 16 * (ch // 2))
                k.wait("dve", s_st[hb], 16 * (ch // 2))
            for q4 in range(4):
                ps = psb[4 * (i % 2) + q4]
                ev_eng = "act" if q4 % 2 == 0 else "dve"
                s_ev = s_eva if ev_eng == "act" else s_evv
                if i >= 2:
                    k.wait("pe", s_ev, 2 * (i - 2) + q4 // 2 + 1)
                for kk4 in range(4):
                    kk = 4 * q4 + kk4
                    k.op("pe", lambda e, ps=ps, kk=kk, kk4=kk4, b=b: e.transpose(ps[:, kk4 * 128:(kk4 + 1) * 128], xn[b][:, kk * 128:(kk + 1) * 128], ident),
                         sig=(s_tp if kk4 == 3 else None))
                k.wait(ev_eng, s_tp)
                for kk4 in range(4):
                    kk = 4 * q4 + kk4
                    dst = hs[hb][:, kk, sub * 128:(sub + 1) * 128]
                    src = ps[:, kk4 * 128:(kk4 + 1) * 128]
                    sg = (s_ev if kk4 == 3 else None)
                    if ev_eng == "act":
                        k.op("act", lambda e, dst=dst, src=src, kk=kk: e.activation(out=dst, in_=src, func=AF.Identity, scale=a1c[:, kk:kk + 1], bias=b1c[:, kk:kk + 1]), sig=sg)
                    else:
                        k.op("dve", lambda e, dst=dst, src=src, kk=kk: e.tensor_scalar(out=dst, in0=src, scalar1=a1c[:, kk:kk + 1], scalar2=b1c[:, kk:kk + 1], op0=ALU.mult, op1=ALU.add), sig=sg)
            if sub == 3:
                k.wait("sync", s_eva)
                k.wait("sync", s_evv)
                k.dma("sync", hTd[:, :, ch * 512:(ch + 1) * 512].rearrange("kk p t -> p kk t"), hs[hb][:], s_st[hb])
        for eng in ENGS:
            k.wait(eng, s_st[0])
            k.wait(eng, s_st[1])
        k.end_stage()


def finish_dbg(c, what):
    k, nc = c["k"], c["nc"]
    dbg = c["dbg"]
    s_o = k.sem("dbg_o")
    if what == "s1":
        k.dma("sync", dbg[0:KD, :, :], c["hTd"][:, :, 2048:2560], s_o)
        k.dma("sync", dbg[KD:KD + 1, 0:16, :].rearrange("o (a b) c -> o a (b c)", a=4), c["modbc"][0:1, :, :], s_o)
    if what == "s2":
        k.dma("pool", dbg[0, :, :], c["kTd"][3, :, :], s_o)
        k.dma("pool", dbg[1, :, 0:TO], c["qTd"][5, :, :], s_o)
        k.dma("pool", dbg[2, :, 0:NH * DH], c["vd"][20, :, :], s_o)
        k.dma("pool", dbg[3, :, 0:HALO + TO], c["u0d"][2, :, :], s_o)
        k.dma("pool", dbg[4, 0:8, :], c["lTd"], s_o)
        k.wait("pool", s_o)
    if what == "s3":
        k.dma("pool", dbg[0], c["conv_outT"][:, :, :], s_o)
        k.wait("pool", s_o)
    if what == "s4":
        k.dma("pool", dbg[0], c["conv_outT"][:, :, :], s_o)
        k.dma("pool", dbg[1], c["attn_outT"][:, :, :], s_o)
        k.wait("pool", s_o)
    if what == "s6":
        k.dma("sync", dbg[0], c["x1d"], s_o)
        k.dma("pool", dbg[1, 0:128, 0:64], c["w4"][:, :, :].rearrange("p a b -> p (a b)"), s_o)
        k.dma("pool", dbg[1, 128:256, 0:64], c["slot4i"][:, :, :].rearrange("p a b -> p (a b)"), s_o)
        k.dma("pool", dbg[1, 256:257, 0:32], c["cntd"], s_o)
        k.wait("pool", s_o)
    k.wait("sync", s_o)
    k.flush()


def make_consts():
    cst = np.zeros((128, 1024), np.float32)
    p = np.arange(128)
    cst[:, 0:128] = np.eye(128, dtype=np.float32)
    cst[:, 128:256] = (p[:, None] <= p[None, :]).astype(np.float32)
    cst[:, 256:384] = (p[:, None] < p[None, :]).astype(np.float32)
    cst[:, 384:512] = 1.0
    cst[:, 512:544] = (np.arange(NE) * CSB)[None, :].astype(np.float32)
    cst[64, 544:672] = 1.0
    sel8 = np.zeros((8, 8 * 128), np.float32)
    for h in range(8):
        sel8[h, h * 128:(h + 1) * 128] = 1.0
    return cst, sel8


def prep_inputs(inp, with_moe=True):
    f = lambda a: np.ascontiguousarray(np.asarray(a, dtype=np.float32))
    x = f(inp["x"])
    cst, sel8 = make_consts()
    shared = {
        "ada_w": f(inp["ada_w"][0]), "ada_b": f(inp["ada_b"][0]).reshape(1, -1),
        "g1T": f(np.asarray(inp["norm_mix_g"][0]).reshape(KD, 128).T),
        "g2": f(inp["norm_ffn_g"][0]).reshape(1, D),
        "w_in": f(inp["w_in"][0]),
        "bfc": f(inp["b_f"][0]).reshape(8, 1),
        "gq": f(inp["q_norm_g"][0]).reshape(128, 1), "gk": f(inp["k_norm_g"][0]).reshape(128, 1),
        "cwT": f(np.asarray(inp["conv_w"][0]).reshape(CK, NG, 128).transpose(2, 1, 0)),
        "cbT": f(np.asarray(inp["conv_b"][0]).reshape(NG, 128).T),
        "lgT": f(np.asarray(inp["conv_ln_g"][0]).reshape(NG, 128).T),
        "lbT": f(np.asarray(inp["conv_ln_b"][0]).reshape(NG, 128).T),
        "w_out": f(inp["w_out"][0]), "w_r": f(inp["w_router"][0]), "b_r": f(inp["b_router"][0]).reshape(1, NE),
        "consts": cst, "sel8": sel8,
    }
    if with_moe:
        shared.update({
            "w_g": f(inp["w_gate"][0]), "w_u": f(inp["w_up"][0]), "w_d": f(inp["w_down"][0]),
            "b_g": f(np.asarray(inp["b_gate"][0]).reshape(NE, KD, 128).transpose(2, 0, 1)),
            "b_u": f(np.asarray(inp["b_up"][0]).reshape(NE, KD, 128).transpose(2, 0, 1)),
            "b_d": f(inp["b_down"][0]),
        })
    maps = []
    for core in range(8):
        b, s = core // 2, core % 2
        meta = np.zeros((128, 64), np.float32)
        if s == 0:
            xvv = np.concatenate([np.zeros((TO, D), np.float32), x[b, :TO]], axis=0)
            meta[:, 0:16] = NEGB
            meta[:, 32] = 0.0
        else:
            xvv = x[b]
            meta[:, 32] = 1.0
        m = dict(shared)
        m["xv"] = np.ascontiguousarray(xvv)
        m["meta"] = meta
        m["cT"] = f(np.asarray(inp["c"][b]).reshape(KD, 128).T)
        maps.append(m)
    return maps


def _input_names(nc):
    return [a.memorylocations[0].name for a in nc.allocations
            if isinstance(a, mybir.MemoryLocationSet) and a.kind == "ExternalInput"]


def kernel(**inputs):
    maps = prep_inputs(inputs, with_moe=True)
    nc = build("full")
    names = _input_names(nc)
    maps = [{n: m[n] for n in names if n in m} for m in maps]
    res = run_bass_kernel_spmd(nc, maps, core_ids=list(range(8)))
    out = np.empty((4, TV, D), np.float32)
    for core in range(8):
        b, s = core // 2, core % 2
        out[b, s * TO:(s + 1) * TO] = res.results[core]["out"]
    return out


class Ring:
    def __init__(self, k, name, n):
        self.k, self.n, self.i, self.name = k, n, 0, name
        self.rel = k.sem(name)
        self.vals = [0] * n
        self.dsem = [None] * n
        self.dvals = [0] * n

    def next(self, *engs):
        slot = self.i % self.n
        for e in engs:
            if self.vals[slot]:
                self.k.wait(e, self.rel, self.vals[slot])
            if self.dvals[slot]:
                self.k.wait(e, self.dsem[slot], self.dvals[slot])
        self.i += 1
        return slot

    def release(self, slot, eng, fn):
        self.vals[slot] = self.k.op(eng, fn, sig=self.rel)
        return self.vals[slot]

    def release_dma(self, slot, eng, out, in_):
        if self.dsem[slot] is None:
            self.dsem[slot] = self.k.sem(f"{self.name}_d{slot}")
        self.dvals[slot] = self.k.dma(eng, out, in_, self.dsem[slot])
        return self.dvals[slot]

    def wait_all(self, eng):
        self.k.wait(eng, self.rel)
        for sl in range(self.n):
            if self.dsem[sl] is not None:
                self.k.wait(eng, self.dsem[sl])


def stage_proj(c):
    k, nc, psb = c["k"], c["nc"], c["psb"]
    hTd, w_in, kTd, qTd, vd, u0d = c["hTd"], c["w_in"], c["kTd"], c["qTd"], c["vd"], c["u0d"]
    ones, metat, gqc, gkc = c["ones"], c["metat"], c["gqc"], c["gkc"]
    lTd = c["lTd"]
    print("sbuf before proj", nc.sbuf_bytes_remaining)
    with contextlib.ExitStack() as st:
        sbt = lambda n, s, d: st.enter_context(nc.sbuf_tensor(n, list(s), d))
        lch = [sbt(f"p_l{i}", [8, 512], F32) for i in range(2)]
        r_l = Ring(k, "p_rl", 2)
        htc = [sbt(f"p_ht{i}", [128, KD, 512], F32R) for i in range(2)]
        wp = [sbt(f"p_wp{i}", [128, KD, 256], F32R) for i in range(3)]
        onesr = sbt("p_onesr", [128, 128], F32R)
        epsc = sbt("p_eps", [128, 1], F32)
        nbf = sbt("p_nbf", [8, 1], F32)
        nbf2 = sbt("p_nbf2", [8, 1], F32)
        sqb = [sbt(f"p_sq{i}", [128, 512], F32R) for i in range(2)]
        lnb = [sbt(f"p_ln{i}", [128, 512], F32) for i in range(2)]
        knb = [sbt(f"p_kn{i}", [128, 512], BF16) for i in range(2)]
        vst = [sbt(f"p_vs{i}", [128, 4, NH * DH], BF16) for i in range(1)]
        sgb = [sbt(f"p_sg{i}", [128, 512], F32) for i in range(2)]
        u0s = [sbt(f"p_u0{i}", [128, 512], F32) for i in range(2)]
        fe = sbt("p_fe", [8, 512], F32)

        s_i = k.sem("p_i")
        k.dma("sync", nbf[:], c["bfc"], s_i)
        k.wait("dve", s_i)
        k.op("dve", lambda e: e.tensor_copy(out=onesr[:], in_=ones))
        k.op("dve", lambda e: e.memset(epsc[:], EPS))
        k.op("dve", lambda e: e.tensor_scalar(out=nbf2[:], in0=nbf[:], scalar1=-1.0, scalar2=None, op0=ALU.mult), sig=s_i)
        for eng in ("pe", "act"):
            k.wait(eng, s_i)
        k.wait("pe", c["s_c0"])

        s_ht = [k.sem("p_ht0"), k.sem("p_ht1")]
        s_wp = [k.sem(f"p_wp{i}") for i in range(3)]
        r_ht = Ring(k, "p_rht", 2)
        r_wp = Ring(k, "p_rwp", 3)
        r_main = Ring(k, "p_rmain", 4)
        r_ss = Ring(k, "p_rss", 2)
        r_sq = Ring(k, "p_rsq", 2)
        r_ln = Ring(k, "p_rln", 2)
        r_kn = Ring(k, "p_rkn", 2)
        r_vs = Ring(k, "p_rvs", 1)
        r_sg = Ring(k, "p_rsg", 2)
        r_u0 = Ring(k, "p_ru0", 2)
        r_f = Ring(k, "p_rf", 1)
        s_pe = k.sem("p_pe")
        s_act = k.sem("p_act")
        s_dve = k.sem("p_dve")
        s_pe2 = k.sem("p_pe2")
        s_act2 = k.sem("p_act2")

        onesb_ = c["onesb"]

        def pe_dummy(e):
            return e.matmul(psb[7][0:1, 0:2], lhsT=onesb_[:, 0:1], rhs=onesb_[:, 0:2], start=True, stop=True)

        pending = []

        def flush_pending():
            while pending:
                pending.pop(0)()

        def load_piece(col_ranges):
            slot = r_wp.next("pool")
            off = 0
            for (c0, c1) in col_ranges:
                v = k.dma("pool", wp[slot][:, :, off:off + (c1 - c0)], w_in[:, c0:c1].rearrange("(kk p) n -> p kk n", p=128), s_wp[slot])
                off += c1 - c0
            k.wait("pe", s_wp[slot], v)
            return slot

        def qk_unit(hslot, wslot, wcol, ntok, gcol, dst_ap):
            bank = r_main.next("pe")
            ps = psb[bank]
            for kk in range(KD):
                k.op("pe", lambda e, kk=kk: e.matmul(ps[:, :ntok], lhsT=wp[wslot][:, kk, wcol:wcol + 128], rhs=htc[hslot][:, kk, 512 - ntok:512], start=(kk == 0), stop=(kk == KD - 1)),
                     sig=(s_pe if kk == KD - 1 else None))
            pv = s_pe.n
            sq = r_sq.next("act")
            k.wait("act", s_pe, pv)
            av = k.op("act", lambda e: e.activation(out=sqb[sq][:, :ntok], in_=ps[:, :ntok], func=AF.Square), sig=s_act)

            def ssum_part():
                sb_ = r_ss.next("pe")
                pss = psb[4 + sb_]
                k.wait("pe", s_act, av)
                sv = r_sq.release(sq, "pe", lambda e: e.matmul(pss[:, :ntok], lhsT=onesr[:, :], rhs=sqb[sq][:, :ntok], start=True, stop=True))
                ln = r_ln.next("act")
                k.wait("act", r_sq.rel, sv)
                a1 = k.op("act", lambda e: e.activation(out=lnb[ln][:, :ntok], in_=pss[:, :ntok], func=AF.Ln, scale=1.0 / DH, bias=epsc[:, 0:1]), sig=s_act2)
                k.wait("act", s_act2, a1)
                a2 = r_ss.release(sb_, "act", lambda e: e.activation(out=lnb[ln][:, :ntok], in_=lnb[ln][:, :ntok], func=AF.Exp, scale=-0.5))
                kn = r_kn.next("dve")
                k.wait("dve", r_ss.rel, a2)
                dv = r_main.release(bank, "dve", lambda e: e.scalar_tensor_tensor(out=knb[kn][:, :ntok], in0=ps[:, :ntok], scalar=gcol[:, 0:1], in1=lnb[ln][:, :ntok], op0=ALU.mult, op1=ALU.mult))
                k.wait("dve", r_main.rel, dv)
                r_ln.release(ln, "dve", lambda e: e.tensor_copy(out=lnb[ln][0:1, 0:1], in_=lnb[ln][0:1, 0:1]))
                k.wait("sync", r_main.rel, dv)
                r_kn.release_dma(kn, "sync", dst_ap, knb[kn][:, :ntok])
            flush_pending()
            pending.append(ssum_part)

        for ci in range(8):
            own = ci >= 4
            hslot = r_ht.next("pool")
            hv_ = k.dma("pool", htc[hslot][:], hTd[:, :, ci * 512:(ci + 1) * 512].rearrange("kk p t -> p kk t"), s_ht[hslot])
            k.wait("pe", s_ht[hslot], hv_)
            last_pe_user = []
            jobs = [("k", i) for i in range(4)] + ([("q", i) for i in range(4)] if own else [])
            for kind, i in jobs:
                base = 1024 if kind == "k" else 0
                wslot = load_piece([(base + 256 * i, base + 256 * (i + 1))])
                for hh in range(2):
                    h = 2 * i + hh
                    if kind == "k":
                        dst = kTd[h, :, ci * 512:(ci + 1) * 512]
                        gcol = gkc
                    else:
                        dst = qTd[h, :, (ci - 4) * 512:(ci - 3) * 512]
                        gcol = gqc
                    qk_unit(hslot, wslot, 128 * hh, 512, gcol, dst)
                r_wp.release(wslot, "pe", pe_dummy)
            flush_pending()
            r_f.next("pe")
            wslot = load_piece([(3072, 3200)])
            for kk in range(KD):
                k.op("pe", lambda e, kk=kk, hslot=hslot, wslot=wslot: e.matmul(psb[6][:, :], lhsT=wp[wslot][:, kk, 0:128], rhs=htc[hslot][:, kk, :], start=(kk == 0), stop=(kk == KD - 1)),
                     sig=(s_pe if kk == KD - 1 else None))
            r_wp.release(wslot, "pe", pe_dummy)
            k.wait("act", s_pe)
            fa = k.op("act", lambda e: e.activation(out=fe[:, :], in_=psb[6][0:8, :], func=AF.Exp, scale=-1.0, bias=nbf2[:, 0:1]), sig=s_act2)
            k.wait("act", s_act2, fa)
            li = r_l.next("act")
            lv = r_f.release(0, "act", lambda e, li=li: e.activation(out=lch[li][:, :], in_=fe[:, :], func=AF.Ln, scale=1.0, bias=ones[0:8, 0:1]))
            k.wait("sync", r_f.rel, lv)
            r_l.release_dma(li, "sync", lTd[:, ci * 512:(ci + 1) * 512], lch[li][:, :])
            vs = r_vs.next("act", "dve")
            for i in range(4):
                wslot = load_piece([(2048 + 256 * i, 2048 + 256 * (i + 1))])
                for tt in range(4):
                    bank = r_main.next("pe")
                    ps = psb[bank]
                    for kk in range(KD):
                        k.op("pe", lambda e, kk=kk, tt=tt, ps=ps, wslot=wslot, hslot=hslot: e.matmul(ps[:, 0:256], lhsT=htc[hslot][:, kk, tt * 128:(tt + 1) * 128], rhs=wp[wslot][:, kk, :], start=(kk == 0), stop=(kk == KD - 1)),
                             sig=(s_pe if kk == KD - 1 else None))
                    k.wait("dve", s_pe)
                    r_main.release(bank, "dve", lambda e, ps=ps, tt=tt, i=i, vs=vs: e.tensor_copy(out=vst[vs][:, tt, 256 * i:256 * (i + 1)], in_=ps[:, 0:256]))
                r_wp.release(wslot, "pe", pe_dummy)
            k.wait("sync", r_main.rel)
            r_vs.release_dma(vs, "sync", vd[4 * ci:4 * ci + 4, :, :].rearrange("t p n -> p t n"), vst[vs][:])
            if ci >= 3:
                ntok = 512 if own else HALO
                ucol = (HALO + (ci - 4) * 512) if own else 0
                for g in range(NG):
                    wslot = load_piece([(3080 + 128 * g, 3080 + 128 * (g + 1)), (4104 + 128 * g, 4104 + 128 * (g + 1))])
                    banks = []
                    for part in range(2):
                        bank = r_main.next("pe")
                        banks.append(bank)
                        ps = psb[bank]
                        for kk in range(KD):
                            k.op("pe", lambda e, kk=kk, ps=ps, part=part, wslot=wslot, hslot=hslot, ntok=ntok: e.matmul(ps[:, :ntok], lhsT=wp[wslot][:, kk, 128 * part:128 * (part + 1)], rhs=htc[hslot][:, kk, 512 - ntok:512], start=(kk == 0), stop=(kk == KD - 1)),
                                 sig=(s_pe if kk == KD - 1 else None))
                    r_wp.release(wslot, "pe", pe_dummy)
                    pa, pg = psb[banks[0]], psb[banks[1]]
                    sgi = r_sg.next("act")
                    k.wait("act", s_pe)
                    sv = k.op("act", lambda e, sgi=sgi, pg=pg, ntok=ntok: e.activation(out=sgb[sgi][:, :ntok], in_=pg[:, :ntok], func=AF.Sigmoid), sig=s_act)
                    ui = r_u0.next("dve")
                    k.wait("dve", s_act, sv)
                    if own:
                        k.op("dve", lambda e, ui=ui, pa=pa, sgi=sgi, ntok=ntok: e.tensor_tensor(out=u0s[ui][:, :ntok], in0=pa[:, :ntok], in1=sgb[sgi][:, :ntok], op=ALU.mult), sig=s_dve)
                    else:
                        k.op("dve", lambda e, ui=ui, pa=pa, sgi=sgi, ntok=ntok: e.scalar_tensor_tensor(out=u0s[ui][:, :ntok], in0=pa[:, :ntok], scalar=metat[:, 32:33], in1=sgb[sgi][:, :ntok], op0=ALU.mult, op1=ALU.mult), sig=s_dve)
                    dv = s_dve.n
                    k.wait("dve", s_dve, dv)
                    r_main.release(banks[0], "dve", lambda e, sgi=sgi: e.tensor_copy(out=sgb[sgi][0:1, 0:1], in_=sgb[sgi][0:1, 0:1]))
                    r_main.release(banks[1], "dve", lambda e, sgi=sgi: e.tensor_copy(out=sgb[sgi][0:1, 1:2], in_=sgb[sgi][0:1, 1:2]))
                    r_sg.release(sgi, "dve", lambda e, sgi=sgi: e.tensor_copy(out=sgb[sgi][0:1, 2:3], in_=sgb[sgi][0:1, 2:3]))
                    k.wait("sync", s_dve, dv)
                    r_u0.release_dma(ui, "sync", u0d[g, :, ucol:ucol + ntok], u0s[ui][:, :ntok])
            r_ht.release(hslot, "pe", pe_dummy)
        fin = k.sem("p_fin")
        for r in (r_kn, r_vs, r_u0, r_l):
            r.wait_all("sync")
        k.wait("sync", r_f.rel)
        k.op("sync", lambda e: e.dma_start(out=c["cntd"][0:1, 0:1], in_=c["cntd"][0:1, 1:2]), sig=fin, inc=16)
        for eng in ENGS:
            k.wait(eng, fin)
        k.end_stage()


def stage_conv(c):
    print("sbuf before conv", c["nc"].sbuf_bytes_remaining)
    k, nc, psb = c["k"], c["nc"], c["psb"]
    u0d, cwT, cbT, lgT, lbT, ones = c["u0d"], c["cwT"], c["cbT"], c["lgT"], c["lbT"], c["ones"]
    conv_outT = c["conv_outT"]
    with contextlib.ExitStack() as st:
        sbt = lambda n, s, d: st.enter_context(nc.sbuf_tensor(n, list(s), d))
        u0 = [sbt(f"c_u{i}", [128, HALO + TO], F32) for i in range(2)]
        v = sbt("c_v", [128, NG, TO], F32)
        cw = sbt("c_cw", [128, NG, CK], F32)
        cb = sbt("c_cb", [128, NG], F32)
        lg = sbt("c_lg", [128, NG], F32)
        lb = sbt("c_lb", [128, NG], F32)
        epsc = sbt("c_eps", [128, 1], F32)
        sqt = [sbt(f"c_sq{i}", [128, 512], F32) for i in range(2)]
        mean = sbt("c_mean", [128, 512], F32)
        msq = sbt("c_msq", [128, 512], F32)
        rstd = sbt("c_rstd", [128, 512], F32)
        yt = [sbt(f"c_y{i}", [128, 512], F32) for i in range(2)]
        s_i = k.sem("c_i")
        k.dma("sync", cw[:], cwT, s_i)
        k.dma("sync", cb[:], cbT, s_i)
        k.dma("sync", lg[:], lgT, s_i)
        k.dma("sync", lb[:], lbT, s_i)
        s_u = [k.sem("c_u0"), k.sem("c_u1")]
        s_cv = k.sem("c_cvd")
        r_u = Ring(k, "c_rud", 2)
        k.op("dve", lambda e: e.memset(epsc[:], EPS))
        for eng in ("dve", "pool", "act"):
            k.wait(eng, s_i)
        gdone = {}
        eng = "dve"
        for g in range(NG):
            b = r_u.next("sync")
            uv = k.dma("sync", u0[b][:], u0d[g, :, :], s_u[b])
            k.wait(eng, s_u[b], uv)
            sc = s_cv
            base = HALO - (CK - 1)
            vv = k.op(eng, lambda e, g=g, b=b: e.tensor_scalar(out=v[:, g, :], in0=u0[b][:, base:base + TO], scalar1=cw[:, g, 0:1], scalar2=cb[:, g:g + 1], op0=ALU.mult, op1=ALU.add), sig=sc)
            for t in range(1, CK):
                k.wait(eng, sc, vv)
                if t < CK - 1:
                    vv = k.op(eng, lambda e, g=g, b=b, t=t: e.scalar_tensor_tensor(out=v[:, g, :], in0=u0[b][:, base + t:base + t + TO], scalar=cw[:, g, t:t + 1], in1=v[:, g, :], op0=ALU.mult, op1=ALU.add), sig=sc)
                else:
                    vv = r_u.release(b, eng, lambda e, g=g, b=b, t=t: e.scalar_tensor_tensor(out=v[:, g, :], in0=u0[b][:, base + t:base + t + TO], scalar=cw[:, g, t:t + 1], in1=v[:, g, :], op0=ALU.mult, op1=ALU.add))
            gdone[g] = (r_u.rel, vv)
        for (sm, vv) in gdone.values():
            for eng in ("pe", "act", "dve"):
                k.wait(eng, sm, vv)
        s_sq = k.sem("c_sq")
        r_sq = Ring(k, "c_rsq", 2)
        s_pe = k.sem("c_pe")
        s_d = k.sem("c_d")
        s_a = k.sem("c_a")
        r_y = Ring(k, "c_ry", 2)
        s_fin = k.sem("c_fin")
        for tc in range(4):
            cs = slice(tc * 512, (tc + 1) * 512)
            ps1, ps2 = psb[2 * (tc % 2)], psb[2 * (tc % 2) + 1]
            if tc >= 2:
                k.wait("pe", s_d, dfree[tc - 2])
            for g in range(NG):
                k.op("pe", lambda e, g=g, ps1=ps1, cs=cs: e.matmul(ps1[:, :], lhsT=ones, rhs=v[:, g, cs], start=(g == 0), stop=(g == NG - 1)))
            for g in range(NG):
                sq = r_sq.next("act")
                av = k.op("act", lambda e, g=g, sq=sq, cs=cs: e.activation(out=sqt[sq][:, :], in_=v[:, g, cs], func=AF.Square), sig=s_sq)
                k.wait("pe", s_sq, av)
                r_sq.release(sq, "pe", lambda e, g=g, sq=sq, ps2=ps2: e.matmul(ps2[:, :], lhsT=ones, rhs=sqt[sq][:, :], start=(g == 0), stop=(g == NG - 1)))
            pv = r_sq.rel.n
            k.wait("dve", r_sq.rel, pv)
            if tc >= 1:
                k.wait("dve", r_y.rel)
            d1 = k.op("dve", lambda e, ps1=ps1: e.tensor_scalar(out=mean[:, :], in0=ps1[:, :], scalar1=1.0 / CW, scalar2=None, op0=ALU.mult), sig=s_d)
            k.wait("dve", s_d, d1)
            d2 = k.op("dve", lambda e: e.tensor_tensor(out=msq[:, :], in0=mean[:, :], in1=mean[:, :], op=ALU.mult), sig=s_d)
            k.wait("dve", s_d, d2)
            d3 = k.op("dve", lambda e, ps2=ps2: e.scalar_tensor_tensor(out=msq[:, :], in0=ps2[:, :], scalar=1.0 / CW, in1=msq[:, :], op0=ALU.mult, op1=ALU.subtract), sig=s_d)
            if tc == 0:
                dfree = {}
            dfree[tc] = d3
            k.wait("act", s_d, d3)
            a1 = k.op("act", lambda e: e.activation(out=rstd[:, :], in_=msq[:, :], func=AF.Ln, scale=1.0, bias=epsc[:, 0:1]), sig=s_a)
            k.wait("act", s_a, a1)
            a2 = k.op("act", lambda e: e.activation(out=rstd[:, :], in_=rstd[:, :], func=AF.Exp, scale=-0.5), sig=s_a)
            k.wait("dve", s_a, a2)
            for g in range(NG):
                yi = r_y.next("dve")
                e1 = k.op("dve", lambda e, g=g, yi=yi, cs=cs: e.tensor_tensor(out=yt[yi][:, :], in0=v[:, g, cs], in1=mean[:, :], op=ALU.subtract), sig=s_d)
                k.wait("dve", s_d, e1)
                e2 = k.op("dve", lambda e, yi=yi: e.tensor_tensor(out=yt[yi][:, :], in0=yt[yi][:, :], in1=rstd[:, :], op=ALU.mult), sig=s_d)
                k.wait("act", s_d, e2)
                r_y.release(yi, "act", lambda e, g=g, yi=yi, cs=cs: e.activation(out=conv_outT[:, g, cs], in_=yt[yi][:, :], func=AF.Silu, scale=lg[:, g:g + 1], bias=lb[:, g:g + 1]))
        for eng in ENGS:
            k.wait(eng, r_y.rel)
        k.end_stage()


def stage_attn(c):
    k, nc, psb = c["k"], c["nc"], c["psb"]
    kTd, vd, qTd, metat, ident, tri = c["kTd"], c["vd"], c["qTd"], c["metat"], c["ident"], c["tri"]
    print("sbuf before attn", nc.sbuf_bytes_remaining)
    onesb, identb = c["onesb"], c["identb"]
    attn_outT = c["attn_outT"]
    with contextlib.ExitStack() as st:
        sbt = lambda n, s, d: st.enter_context(nc.sbuf_tensor(n, list(s), d))
        cumT = sbt("a_cum", [8, TV], F32)
        lT = sbt("a_lT", [8, TV], F32)
        cumtok = sbt("a_ctok", [128, NT, NH], F32)
        refbc = sbt("a_ref", [128, NTO, NH], F32)
        tmpb = sbt("a_tmpb", [128, NT], F32)
        biasall = sbt("a_bias", [128, NH, NT, NTO], F32)
        maskb = sbt("a_mask", [128, 128], BF16)
        kT = [sbt(f"a_k{i}", [128, TV], BF16) for i in range(2)]
        vh = [sbt(f"a_v{i}", [128, NT, DH], BF16) for i in range(2)]
        qT = [sbt(f"a_q{i}", [128, TO], BF16) for i in range(2)]
        NP = 4
        pt = [sbt(f"a_p{i}", [128, 128], BF16) for i in range(NP)]
        rz = [sbt(f"a_rz{i}", [128, 128], F32) for i in range(2)]
        s_i = k.sem("a_i")
        s_v = k.sem("a_v")
        s_p = k.sem("a_pe0")
        k.dma("sync", lT[:, :], c["lTd"], s_i)
        k.wait("dve", s_i)
        v1 = k.op("dve", lambda e: e.tensor_tensor_scan(out=cumT[:, :], data0=lT[:, :], data1=lT[:, :], initial=0.0, op0=ALU.add, op1=ALU.max), sig=s_v)
        v2 = k.op("dve", lambda e: e.tensor_scalar(out=maskb[:, :], in0=tri, scalar1=-1.0, scalar2=-NEGB, op0=ALU.add, op1=ALU.mult), sig=s_v)
        k.wait("pe", s_v, v2)
        for kb in range(NT):
            k.op("pe", lambda e, kb=kb: e.transpose(psb[0][:, kb * 8:(kb + 1) * 8], cumT[0:8, kb * 128:(kb + 1) * 128], ident[0:8, 0:8]),
                 sig=(s_p if kb == NT - 1 else None))
        k.wait("dve", s_p)
        v3 = k.op("dve", lambda e: e.tensor_copy(out=cumtok[:].rearrange("p a b -> p (a b)"), in_=psb[0][:, 0:NT * NH]), sig=s_v)
        k.wait("pe", s_v, v3)
        pr = k.op("pe", lambda e: e.matmul(psb[1][:, 0:NTO * NH], lhsT=c["e64"], rhs=cumtok[:, NTO:NT, :].rearrange("p a b -> p (a b)"), start=True, stop=True), sig=s_p)
        k.op("pe", lambda e: e.matmul(psb[7][0:1, 0:2], lhsT=onesb[:, 0:1], rhs=onesb[:, 0:2], start=True, stop=True))
        k.wait("dve", s_p, pr)
        v4 = k.op("dve", lambda e: e.tensor_copy(out=refbc[:].rearrange("p a b -> p (a b)"), in_=psb[1][:, 0:NTO * NH]), sig=s_v)
        k.wait("dve", s_v, v4)
        for h in range(NH):
            v5 = k.op("dve", lambda e, h=h: e.tensor_tensor(out=tmpb[:, :], in0=cumtok[:, :, h], in1=metat[:, 0:NT], op=ALU.add), sig=s_v)
            k.wait("dve", s_v, v5)
            v6 = k.op("dve", lambda e, h=h: e.tensor_tensor(out=biasall[:, h, :, :], in0=tmpb[:, :].unsqueeze(2).to_broadcast([128, NT, NTO]),
                                                          in1=refbc[:, :, h].unsqueeze(1).to_broadcast([128, NT, NTO]), op=ALU.subtract), sig=s_v)
            k.wait("dve", s_v, v6)
        k.wait("act", s_v, v6)
        k.wait("pe", s_v, v6)

        s_kv = [k.sem("a_kv0"), k.sem("a_kv1")]
        r_kv = Ring(k, "a_rkv", 2)
        r_S = Ring(k, "a_rS", 5)
        r_P = Ring(k, "a_rP", NP)
        r_O = Ring(k, "a_rO", 2)
        s_S = k.sem("a_S")
        s_E = k.sem("a_E")
        s_O = k.sem("a_O")
        s_dz = k.sem("a_dz")

        def sslot(i):
            return psb[2 + i][:, 0:128]

        LOOK = globals().get('LOOK_RUN', 2)
        for h in range(globals().get('NH_RUN', NH)):
            hb = r_kv.next("sync")
            k.dma("sync", kT[hb][:], kTd[h, :, :], s_kv[hb])
            for q in range(4):
                k.dma("sync", vh[hb][:, 8 * q:8 * (q + 1), :], vd[8 * q:8 * (q + 1), :, h * DH:(h + 1) * DH].rearrange("t p n -> p t n"), s_kv[hb])
            kvv = k.dma("sync", qT[hb][:], qTd[h, :, :], s_kv[hb])
            k.wait("pe", s_kv[hb], kvv)
            seq = [(j, kb) for j in range(globals().get('NTO_RUN', NTO)) for kb in range(NTO + 1 + j)]
            state = {}
            inflight = []

            def emit_S(j, kb, h=h, hb=hb):
                si = r_S.next("pe")
                diag = (kb == NTO + j)
                sv = k.op("pe", lambda e: e.matmul(sslot(si), lhsT=kT[hb][:, kb * 128:(kb + 1) * 128], rhs=qT[hb][:, j * 128:(j + 1) * 128], start=True, stop=not diag),
                          sig=(None if diag else s_S))
                if diag:
                    sv = k.op("pe", lambda e: e.matmul(sslot(si), lhsT=identb[:, :], rhs=maskb[:, :], start=False, stop=True), sig=s_S)
                pi = r_P.next("act")
                k.wait("act", s_S, sv)
                ev = r_S.release(si, "act", lambda e: e.activation(out=pt[pi][:, :], in_=sslot(si), func=AF.Exp, bias=biasall[:, h, kb, j:j + 1], scale=1.0))
                return (j, kb, pi, ev)

            def emit_PV(j, kb, pi, ev, h=h, hb=hb):
                nk = NTO + 1 + j
                if kb == 0:
                    ob = r_O.next("pe")
                    state["ob"] = ob
                ob = state["ob"]
                pO = psb[ob][:, 0:128]
                pZ = psb[ob][:, 128:256]
                k.wait("pe", r_S.rel, ev)
                k.op("pe", lambda e: e.matmul(pO, lhsT=vh[hb][:, kb, :], rhs=pt[pi][:, :], start=(kb == 0), stop=(kb == nk - 1)))
                last = (kb == nk - 1)
                if not last:
                    r_P.release(pi, "pe", lambda e: e.matmul(pZ, lhsT=onesb[:, :], rhs=pt[pi][:, :], start=(kb == 0), stop=False))
                else:
                    ov = r_P.release(pi, "pe", lambda e: e.matmul(pZ, lhsT=onesb[:, :], rhs=pt[pi][:, :], start=(kb == 0), stop=True))
                    zi = j % 2
                    k.wait("dve", r_P.rel, ov)
                    z1 = k.op("dve", lambda e: e.reciprocal(out=rz[zi][:, :], in_=pZ), sig=s_dz)
                    k.wait("dve", s_dz, z1)
                    r_O.release(ob, "dve", lambda e: e.tensor_tensor(out=attn_outT[:, h, j * 128:(j + 1) * 128], in0=pO, in1=rz[zi][:, :], op=ALU.mult))

            for idx, (j, kb) in enumerate(seq):
                inflight.append(emit_S(j, kb))
                if len(inflight) > LOOK:
                    emit_PV(*inflight.pop(0))
            while inflight:
                emit_PV(*inflight.pop(0))
            r_kv.release(hb, "pe", lambda e: e.matmul(psb[7][0:1, 0:2], lhsT=onesb[:, 0:1], rhs=onesb[:, 0:2], start=True, stop=True))
        for eng in ENGS:
            k.wait(eng, r_O.rel)
            k.wait(eng, r_kv.rel)
        k.end_stage()


def chain(k, eng, sem, fns):
    v = None
    for fn in fns:
        if v is not None:
            k.wait(eng, sem, v)
        v = k.op(eng, fn, sig=sem)
    return v


def stage_outproj(c):
    print("sbuf before outproj", c["nc"].sbuf_bytes_remaining)
    k, nc, psb = c["k"], c["nc"], c["psb"]
    xv, w_out, x1d, modbc = c["xv"], c["w_out"], c["x1d"], c["modbc"]
    attn_outT, conv_outT = c["attn_outT"], c["conv_outT"]
    with contextlib.ExitStack() as st:
        sbt = lambda n, s, d: st.enter_context(nc.sbuf_tensor(n, list(s), d))
        wst = [sbt(f"o_ws{i}", [128, KD, 256], F32) for i in range(2)]
        wob = [sbt(f"o_wb{i}", [128, KD, 512], BF16) for i in range(2)]
        xp = [sbt(f"o_xp{i}", [128, 512], F32) for i in range(4)]
        tp = [sbt(f"o_tp{i}", [128, 512], F32) for i in range(2)]
        op_ = [sbt(f"o_op{i}", [128, 512], F32) for i in range(3)]
        s_ws = [k.sem("o_ws0"), k.sem("o_ws1")]
        r_ws = Ring(k, "o_rws", 2)
        r_wb = Ring(k, "o_rwb", 2)
        s_cast = k.sem("o_cast")
        s_x = [k.sem(f"o_x{i}") for i in range(4)]
        r_x = Ring(k, "o_rx", 4)
        r_ps = Ring(k, "o_rps", 4)
        r_o = Ring(k, "o_ro", 3)
        s_pe = k.sem("o_pe")
        s_d = k.sem("o_d")
        onesb = c["onesb"]
        for dg in range(4):
            wb = r_wb.next("dve")
            for half in range(2):
                ws = r_ws.next("sync")
                wv = k.dma("sync", wst[ws][:], w_out[:, dg * 512 + half * 256: dg * 512 + (half + 1) * 256].rearrange("(kk p) n -> p kk n", p=128), s_ws[ws])
                k.wait("dve", s_ws[ws], wv)
                cv = r_ws.release(ws, "dve", lambda e, ws=ws, wb=wb, half=half: e.tensor_copy(out=wob[wb][:, :, half * 256:(half + 1) * 256], in_=wst[ws][:, :, :]))
            k.wait("pe", r_ws.rel, cv)
            for i in range(NTO):
                xi = r_x.next("sync")
                xvv = k.dma("sync", xp[xi][:], xv[TO + i * 128:TO + (i + 1) * 128, dg * 512:(dg + 1) * 512], s_x[xi])
                bank = r_ps.next("pe")
                ps = psb[bank]
                for ch in range(16):
                    src = attn_outT if ch < 8 else conv_outT
                    k.op("pe", lambda e, ch=ch, src=src, ps=ps, i=i, wb=wb: e.matmul(ps[:, :], lhsT=src[:, ch % 8, i * 128:(i + 1) * 128], rhs=wob[wb][:, ch, :], start=(ch == 0), stop=(ch == 15)),
                         sig=(s_pe if ch == 15 else None))
                pv = s_pe.n
                ti = (dg * NTO + i) % 2
                oi = r_o.next("dve")
                k.wait("dve", s_pe, pv)
                k.wait("dve", s_x[xi], xvv)
                d1 = r_ps.release(bank, "dve", lambda e, ps=ps, ti=ti, dg=dg: e.tensor_tensor(out=tp[ti][:, :], in0=ps[:, :], in1=modbc[:, 0, dg * 512:(dg + 1) * 512], op=ALU.mult))
                k.wait("dve", r_ps.rel, d1)
                d2 = r_x.release(xi, "dve", lambda e, ti=ti, xi=xi, oi=oi: e.tensor_tensor(out=op_[oi][:, :], in0=tp[ti][:, :], in1=xp[xi][:, :], op=ALU.add))
                k.wait("sync", r_x.rel, d2)
                r_o.release_dma(oi, "sync", x1d[i * 128:(i + 1) * 128, dg * 512:(dg + 1) * 512], op_[oi][:, :])
            r_wb.release(wb, "pe", lambda e: e.matmul(psb[7][0:1, 0:2], lhsT=onesb[:, 0:1], rhs=onesb[:, 0:2], start=True, stop=True))
        for eng in ENGS:
            r_o.wait_all(eng)
            k.wait(eng, r_wb.rel)
        k.end_stage()


def stage_route(c):
    print("sbuf before route", c["nc"].sbuf_bytes_remaining)
    k, nc, psb = c["k"], c["nc"], c["psb"]
    x1d, modbc, w_r, b_r, xs, cntd = c["x1d"], c["modbc"], c["w_r"], c["b_r"], c["xs"], c["cntd"]
    ident, ones, ustr, ebase = c["ident"], c["ones"], c["ustr"], c["ebase"]
    w4, slot4i, WdT = c["w4"], c["slot4i"], c["WdT"]
    with contextlib.ExitStack() as st:
        sbt = lambda n, s, d: st.enter_context(nc.sbuf_tensor(n, list(s), d))
        x1 = [sbt(f"r_x{i}", [128, D], F32) for i in range(2)]
        h2 = sbt("r_h2", [128, D], F32)
        h2b = [sbt(f"r_hb{i}", [128, D], BF16) for i in range(2)]
        h2T = sbt("r_h2T", [128, KD, 128], F32)
        wr = sbt("r_wr", [128, KD, NE], F32)
        brb = sbt("r_br", [128, NE], F32)
        ssq = sbt("r_ssq", [128, NTO], F32)
        rs = sbt("r_rs", [128, NTO], F32)
        lg = sbt("r_lg", [128, NE], F32)
        mx = sbt("r_mx", [128, 8], F32)
        nm = sbt("r_nm", [128, 1], F32)
        e4 = sbt("r_e4", [128, 4], F32)
        es = sbt("r_es", [128, 1], F32)
        mask = sbt("r_mask", [128, NE], F32)
        carry = sbt("r_carry", [128, NE], F32)
        slotd = sbt("r_slotd", [128, NE], F32)
        oh = sbt("r_oh", [128, NE], F32)
        tmp = sbt("r_tmp", [128, NE], F32)
        wd = sbt("r_wd", [128, NE], F32)
        s4f = sbt("r_s4f", [128, 4], F32)
        cnti = sbt("r_cnti", [1, NE], I32)
        s_i = k.sem("r_i")
        k.dma("sync", wr[:], w_r.rearrange("(kk p) n -> p kk n", p=128), s_i)
        k.dma("sync", brb[:], b_r.partition_broadcast(128), s_i)
        k.op("dve", lambda e: e.memset(carry[:], 0.0))
        for eng in ("pe", "dve"):
            k.wait(eng, s_i)
        s_x = [k.sem("r_x0"), k.sem("r_x1")]
        s_a = k.sem("r_a")
        s_d = k.sem("r_d")
        s_pe = k.sem("r_pe")
        s_sc = [k.sem("r_sc0"), k.sem("r_sc1")]
        s_hb = k.sem("r_hb")
        for i in range(NTO):
            b = i % 2
            if i >= 2:
                k.wait("sync", s_d, xfree[i - 2])
            xvv = k.dma("sync", x1[b][:], x1d[i * 128:(i + 1) * 128, :], s_x[b])
            k.wait("act", s_x[b], xvv)
            if i >= 1:
                k.wait("act", s_pe, h2free[i - 1])
            a1 = k.op("act", lambda e, b=b, i=i: e.activation(out=h2[:, :], in_=x1[b][:, :], func=AF.Square, accum_out=ssq[:, i:i + 1]), sig=s_a)
            k.wait("dve", s_a, a1)
            d1 = k.op("dve", lambda e, i=i: e.tensor_scalar(out=rs[:, i:i + 1], in0=ssq[:, i:i + 1], scalar1=1.0 / D, scalar2=EPS, op0=ALU.mult, op1=ALU.add), sig=s_d)
            k.wait("act", s_d, d1)
            a2 = k.op("act", lambda e, i=i: e.activation(out=rs[:, i:i + 1], in_=rs[:, i:i + 1], func=AF.Sqrt), sig=s_a)
            k.wait("dve", s_a, a2)
            d4 = chain(k, "dve", s_d, [
                lambda e, i=i: e.reciprocal(out=rs[:, i:i + 1], in_=rs[:, i:i + 1]),
                lambda e, i=i, b=b: e.scalar_tensor_tensor(out=h2[:, :], in0=x1[b][:, :], scalar=rs[:, i:i + 1], in1=modbc[:, 1, :], op0=ALU.mult, op1=ALU.mult),
                lambda e: e.tensor_tensor(out=h2[:, :], in0=h2[:, :], in1=modbc[:, 2, :], op=ALU.add),
            ])
            if i == 0:
                xfree, h2free = {}, {}
            xfree[i] = d4
            k.wait("act", s_d, d4)
            if i >= 2:
                k.wait("act", s_sc[b], 64 * (i // 2))
            hb = k.op("act", lambda e, b=b: e.activation(out=h2b[b][:, :], in_=h2[:, :], func=AF.Copy), sig=s_hb)
            k.wait("pe", s_d, d4)
            for q4 in range(4):
                for kk4 in range(4):
                    kk = 4 * q4 + kk4
                    k.op("pe", lambda e, q4=q4, kk4=kk4, kk=kk: e.transpose(psb[q4][:, kk4 * 128:(kk4 + 1) * 128], h2[:, kk * 128:(kk + 1) * 128], ident),
                         sig=(s_pe if kk == 15 else None))
            tv = s_pe.n
            h2free[i] = tv
            k.wait("act", s_pe, tv)
            for q4 in range(4):
                av = k.op("act", lambda e, q4=q4: e.activation(out=h2T[:, 4 * q4:4 * q4 + 4, :].rearrange("p a b -> p (a b)"), in_=psb[q4][:, :], func=AF.Copy), sig=(s_a if q4 == 3 else None))
            k.wait("pe", s_a, av)
            for kk in range(KD):
                k.op("pe", lambda e, kk=kk: e.matmul(psb[4][:, 0:NE], lhsT=h2T[:, kk, :], rhs=wr[:, kk, :], start=(kk == 0), stop=(kk == KD - 1)), sig=(s_pe if kk == KD - 1 else None))
            lv = s_pe.n
            k.wait("dve", s_pe, lv)
            dm = chain(k, "dve", s_d, [
                lambda e: e.tensor_tensor(out=lg[:, :], in0=psb[4][:, 0:NE], in1=brb[:, :], op=ALU.add),
                lambda e: e.max(out=mx[:, :], in_=lg[:, :]),
                lambda e: e.tensor_scalar(out=mask[:, :], in0=lg[:, :], scalar1=mx[:, 3:4], scalar2=None, op0=ALU.is_ge),
                lambda e: e.tensor_scalar(out=nm[:, :], in0=mx[:, 0:1], scalar1=-1.0, scalar2=None, op0=ALU.mult),
            ])
            k.wait("pe", s_d, dm)
            k.op("pe", lambda e: e.matmul(psb[5][:, 0:NE], lhsT=ustr, rhs=mask[:, :], start=True, stop=True))
            rv = k.op("pe", lambda e: e.matmul(psb[5][:, NE:2 * NE], lhsT=ones, rhs=mask[:, :], start=True, stop=True), sig=s_pe)
            k.wait("act", s_d, dm)
            ev = k.op("act", lambda e: e.activation(out=e4[:, :], in_=mx[:, 0:4], func=AF.Exp, bias=nm[:, 0:1], scale=1.0, accum_out=es[:, 0:1]), sig=s_a)
            k.wait("dve", s_a, ev)
            k.wait("dve", s_pe, rv)
            fns = [
                lambda e: e.reciprocal(out=es[:, :], in_=es[:, :]),
                lambda e, i=i: e.tensor_scalar(out=w4[:, i, :], in0=e4[:, :], scalar1=es[:, 0:1], scalar2=None, op0=ALU.mult),
                lambda e: e.tensor_tensor(out=slotd[:, :], in0=psb[5][:, 0:NE], in1=carry[:, :], op=ALU.add),
                lambda e: e.tensor_scalar(out=slotd[:, :], in0=slotd[:, :], scalar1=float(CSB - 1), scalar2=None, op0=ALU.min),
                lambda e: e.tensor_tensor(out=slotd[:, :], in0=slotd[:, :], in1=ebase, op=ALU.add),
                lambda e: e.tensor_tensor(out=carry[:, :], in0=carry[:, :], in1=psb[5][:, NE:2 * NE], op=ALU.add),
            ]
            for kq in range(4):
                fns += [
                    lambda e, kq=kq: e.tensor_scalar(out=oh[:, :], in0=lg[:, :], scalar1=mx[:, kq:kq + 1], scalar2=None, op0=ALU.is_equal),
                    lambda e: e.tensor_tensor(out=tmp[:, :], in0=oh[:, :], in1=slotd[:, :], op=ALU.mult),
                    lambda e, kq=kq: e.tensor_reduce(out=s4f[:, kq:kq + 1], in_=tmp[:, :], axis=AX.X, op=ALU.add),
                ]
                if kq == 0:
                    fns.append(lambda e, i=i: e.tensor_scalar(out=wd[:, :], in0=oh[:, :], scalar1=w4[:, i, 0:1], scalar2=None, op0=ALU.mult))
                else:
                    fns.append(lambda e, i=i, kq=kq: e.scalar_tensor_tensor(out=wd[:, :], in0=oh[:, :], scalar=w4[:, i, kq:kq + 1], in1=wd[:, :], op0=ALU.mult, op1=ALU.add))
            fns.append(lambda e, i=i: e.tensor_copy(out=slot4i[:, i, :], in_=s4f[:, :]))
            dz = chain(k, "dve", s_d, fns)
            k.wait("pe", s_d, dz)
            tw = k.op("pe", lambda e: e.transpose(psb[6][0:NE, 0:128], wd[:, :], ident), sig=s_pe)
            k.wait("dve", s_pe, tw)
            k.wait("dve", s_d, dz)
            dw = k.op("dve", lambda e, i=i: e.tensor_copy(out=WdT[:, i, :], in_=psb[6][0:NE, 0:128]), sig=s_d)
            k.wait("pool", s_d, dz)
            k.wait("pool", s_hb, hb)
            if i == 0:
                k.wait("pool", c["s_z"])
            for kq in range(4):
                k.op("pool", lambda e, b=b, i=i, kq=kq: e.indirect_dma_start(
                    out=xs, out_offset=bass.IndirectOffsetOnAxis(ap=slot4i[:, i, kq:kq + 1], axis=0),
                    in_=h2b[b][:, :], in_offset=None), sig=s_sc[b], inc=16)
        k.wait("dve", s_d, dw)
        cz = k.op("dve", lambda e: e.tensor_copy(out=cnti[:, :], in_=carry[0:1, :]), sig=s_d)
        k.wait("sync", s_d, cz)
        fin = k.sem("r_fin")
        k.dma("sync", cntd[:, :], cnti[:, :], fin)
        k.wait("sync", s_sc[0])
        k.wait("sync", s_sc[1])
        k.wait("sync", fin)
        k.op("sync", lambda e: e.dma_start(out=cntd[0:1, 0:1], in_=cntd[0:1, 0:1]), sig=fin, inc=16)
        for eng in ENGS:
            k.wait(eng, fin)
        k.end_stage()


USE_IF = True


class Guard:
    def __init__(self, k, c, engs, thr):
        self.k, self.c, self.engs, self.thr = k, c, engs, thr

    def __enter__(self):
        k = self.k
        self.saved = {e: k.q[e] for e in self.engs}
        for e in self.engs:
            k.q[e] = []
        self.sig0 = None
        k._rec = {e: [] for e in self.engs}
        return self

    def __exit__(self, *a):
        k, c, thr = self.k, self.c, self.thr
        for eng in self.engs:
            body = k.q[eng]
            k.q[eng] = self.saved[eng]
            if not body:
                continue
            sigs = k._rec[eng]
            if not USE_IF:
                k.q[eng].extend(body)
                continue
            dummy = c["dummy"][eng]
            tot = {}
            for (h, inc, so) in sigs:
                tot.setdefault(id(so), [so, 0])[1] += inc
            fences = [(so.h, so.n - t) for (so, t) in tot.values() if so.n - t > 0]

            def f(e, body=body, sigs=sigs, eng=eng, dummy=dummy, thr=thr, fences=fences):
                n = k.cur[eng]
                with e.If(n > thr):
                    for g in body:
                        g(e)
                if sigs:
                    with e.Else():
                        for (h, v) in fences:
                            e.wait_ge(h, v)
                        for (h, inc, _so) in sigs:
                            dummy(e).then_inc(h, inc)
            k.q[eng].append(f)
        k._rec = None
        return False


def _kb_op_rec(self, eng, fn, sig=None, inc=1):
    if sig is not None and getattr(self, "_rec", None) is not None and eng in self._rec:
        self._rec[eng].append((sig.h, inc, sig))
    return KB._op_orig(self, eng, fn, sig=sig, inc=inc)


KB._op_orig = KB.op
KB.op = _kb_op_rec
KB._rec = None
KB.cur = None


def stage_moe(c):
    print("sbuf before moe", c["nc"].sbuf_bytes_remaining)
    k, nc, psb = c["k"], c["nc"], c["psb"]
    xs, osd, cntd = c["xs"], c["osd"], c["cntd"]
    w_g, w_u, w_d, b_g, b_u = c["w_g"], c["w_u"], c["w_d"], c["b_g"], c["b_u"]
    identb, onesb = c["identb"], c["onesb"]
    with contextlib.ExitStack() as st:
        sbt = lambda n, s, d: st.enter_context(nc.sbuf_tensor(n, list(s), d))
        xst = [sbt(f"e_xs{i}", [128, 2, D], BF16) for i in range(2)]
        xbT = sbt("e_xbT", [128, KD, CSB], BF16)
        hidT = sbt("e_hidT", [128, KD, CSB], BF16)
        wgb = [sbt(f"e_wg{i}", [128, KD, 128], BF16) for i in range(3)]
        wub = [sbt(f"e_wu{i}", [128, KD, 128], BF16) for i in range(3)]
        wdb = [sbt(f"e_wd{i}", [128, KD, 512], BF16) for i in range(2)]
        bg = sbt("e_bg", [128, NE, KD], F32)
        bu = sbt("e_bu", [128, NE, KD], F32)
        gp = [sbt(f"e_gp{i}", [128, TS], F32) for i in range(2)]
        sg = [sbt(f"e_sg{i}", [128, TS], F32) for i in range(2)]
        u1 = [sbt(f"e_u1{i}", [128, TS], F32) for i in range(2)]
        ob = [sbt(f"e_ob{i}", [128, 512], F32) for i in range(4)]
        scr = sbt("e_scr", [128, 8], F32)
        s_i = k.sem("e_i")
        k.dma("sync", bg[:], b_g, s_i)
        k.dma("sync", bu[:], b_u, s_i)
        for eng in ("act", "dve"):
            k.wait(eng, s_i)
        psbb = [p.bitcast(BF16) if hasattr(p, "bitcast") else p for p in psb]

        c["dummy"] = {
            "pe": lambda e: e.matmul(psb[7][0:1, 0:2], lhsT=onesb[:, 0:1], rhs=onesb[:, 0:2], start=True, stop=True),
            "act": lambda e: e.activation(out=scr[0:1, 0:1], in_=scr[0:1, 1:2], func=AF.Copy),
            "dve": lambda e: e.tensor_copy(out=scr[0:1, 2:3], in_=scr[0:1, 3:4]),
            "sync": lambda e: e.dma_start(out=cntd[0:1, NE - 1:NE], in_=cntd[0:1, NE - 1:NE]),
        }
        k.op("dve", lambda e: e.memset(scr[:], 0.0))
        k.cur = {}
        regs = {}

        def load_count(eng, ei):
            def f(e, eng=eng, ei=ei):
                if eng not in regs:
                    regs[eng] = e.alloc_register(f"cnt_{eng}")
                e.reg_load(regs[eng], cntd[0:1, ei:ei + 1])
                k.cur[eng] = e.snap(regs[eng])
            k.q[eng].append(f)

        s_xs = [k.sem("e_xs0"), k.sem("e_xs1")]
        r_xs = Ring(k, "e_rxs", 2)
        r_tp = Ring(k, "e_rtp", 3)
        s_tp = k.sem("e_tp")
        s_xb = k.sem("e_xb")
        s_wg = [k.sem(f"e_wg{i}") for i in range(3)]
        s_wu = [k.sem(f"e_wu{i}") for i in range(3)]
        r_wg = Ring(k, "e_rwg", 3)
        r_wu = Ring(k, "e_rwu", 3)
        s_wd = [k.sem("e_wd0"), k.sem("e_wd1")]
        r_wd = Ring(k, "e_rwd", 2)
        r_gu = Ring(k, "e_rgu", 4)
        s_gu = k.sem("e_gu")
        s_gp = k.sem("e_gp")
        s_sg = k.sem("e_sg")
        s_u1 = k.sem("e_u1")
        s_hid = k.sem("e_hid")
        r_et = Ring(k, "e_ret", 2)
        r_dn = Ring(k, "e_rdn", 4)
        s_dn = k.sem("e_dn")
        r_ob = Ring(k, "e_rob", 4)
        s_ob = k.sem("e_ob")
        s_gud = k.sem("e_gud")
        s_dnd = k.sem("e_dnd")
        gud_val = {}
        dnd_val = {}
        hid_last = {}

        for ei in range(globals().get('NE_RUN', NE)):
            for eng in ("pe", "act", "dve", "sync"):
                load_count(eng, ei)
            if ei >= 1:
                k.wait("act", s_gud, gud_val[ei - 1])
            for j in range(NJ):
                with Guard(k, c, ("sync", "pe", "act"), TS * j):
                    xi = r_xs.next("sync")
                    base = ei * CSB + j * TS
                    xv_ = k.dma("sync", xst[xi][:], xs[base:base + TS, :].rearrange("(h p) d -> p h d", p=128), s_xs[xi])
                    k.wait("pe", s_xs[xi], xv_)
                    for q4 in range(4):
                        bank = r_tp.next("pe")
                        for kk4 in range(4):
                            kk = 4 * q4 + kk4
                            for hh in range(2):
                                last = (kk4 == 3 and hh == 1)
                                fn = lambda e, bank=bank, kk4=kk4, hh=hh, xi=xi, kk=kk: e.transpose(
                                    psbb[4 + bank][:, kk4 * 256 + hh * 128: kk4 * 256 + (hh + 1) * 128], xst[xi][:, hh, kk * 128:(kk + 1) * 128], identb[:, :])
                                if last and q4 == 3:
                                    tv = r_xs.release(xi, "pe", fn)
                                    k.wait("act", r_xs.rel, tv)
                                elif last:
                                    tv = k.op("pe", fn, sig=s_tp)
                                    k.wait("act", s_tp, tv)
                                else:
                                    k.op("pe", fn)
                        r_tp.release(bank, "act", lambda e, bank=bank, q4=q4, j=j: e.activation(
                            out=xbT[:, 4 * q4:4 * q4 + 4, j * TS:(j + 1) * TS], in_=psbb[4 + bank][:, 0:1024].rearrange("p (a b) -> p a b", b=TS), func=AF.Copy))
                    k.op("act", lambda e: e.activation(out=scr[0:1, 4:5], in_=scr[0:1, 5:6], func=AF.Copy), sig=s_xb)
            xb_ready = s_xb.n
            k.wait("pe", s_xb, xb_ready)
            if ei >= 1:
                k.wait("dve", s_dnd, dnd_val[ei - 1])
            k.wait("pe", r_dn.rel)
            for fc in range(KD):
                gi = r_wg.next("pool")
                gv = k.dma("pool", wgb[gi][:], w_g[ei, :, fc * 128:(fc + 1) * 128].rearrange("(kk p) n -> p kk n", p=128), s_wg[gi])
                ui = r_wu.next("pool")
                uv = k.dma("pool", wub[ui][:], w_u[ei, :, fc * 128:(fc + 1) * 128].rearrange("(kk p) n -> p kk n", p=128), s_wu[ui])
                k.wait("pe", s_wg[gi], gv)
                k.wait("pe", s_wu[ui], uv)
                for j in range(NJ):
                    with Guard(k, c, ("pe", "act", "dve"), TS * j):
                        bank = r_gu.next("pe")
                        ps = psb[bank]
                        cs = slice(j * TS, (j + 1) * TS)
                        for kk in range(KD):
                            k.op("pe", lambda e, kk=kk, ps=ps, gi=gi, cs=cs: e.matmul(ps[:, 0:TS], lhsT=wgb[gi][:, kk, :], rhs=xbT[:, kk, cs], start=(kk == 0), stop=(kk == KD - 1)))
                        for kk in range(KD):
                            k.op("pe", lambda e, kk=kk, ps=ps, ui=ui, cs=cs: e.matmul(ps[:, TS:2 * TS], lhsT=wub[ui][:, kk, :], rhs=xbT[:, kk, cs], start=(kk == 0), stop=(kk == KD - 1)),
                                 sig=(s_gu if kk == KD - 1 else None))
                        pv = s_gu.n
                        ti = r_et.next("dve", "act")
                        k.wait("dve", s_gu, pv)
                        k.wait("act", s_gu, pv)
                        d1 = k.op("dve", lambda e, ps=ps, ti=ti, ei=ei, fc=fc: e.tensor_scalar(out=gp[ti][:, :], in0=ps[:, 0:TS], scalar1=bg[:, ei, fc:fc + 1], scalar2=SWIGLU_LIMIT, op0=ALU.add, op1=ALU.min), sig=s_gp)
                        k.wait("act", s_gp, d1)
                        a1 = k.op("act", lambda e, ps=ps, ti=ti, ei=ei, fc=fc: e.activation(out=u1[ti][:, :], in_=ps[:, TS:2 * TS], func=AF.Identity, bias=bu[:, ei, fc:fc + 1], scale=1.0), sig=s_u1)
                        a2 = k.op("act", lambda e, ti=ti: e.activation(out=sg[ti][:, :], in_=gp[ti][:, :], func=AF.Sigmoid, scale=SWIGLU_ALPHA), sig=s_sg)
                        r_gu.release(bank, "act", lambda e: e.activation(out=scr[0:1, 6:7], in_=scr[0:1, 7:8], func=AF.Copy))
                        k.wait("dve", s_u1, a1)
                        d2 = k.op("dve", lambda e, ti=ti: e.tensor_scalar(out=u1[ti][:, :], in0=u1[ti][:, :], scalar1=-SWIGLU_LIMIT, scalar2=SWIGLU_LIMIT, op0=ALU.max, op1=ALU.min), sig=s_gp)
                        k.wait("dve", s_sg, a2)
                        d3 = k.op("dve", lambda e, ti=ti: e.tensor_tensor(out=gp[ti][:, :], in0=gp[ti][:, :], in1=sg[ti][:, :], op=ALU.mult), sig=s_gp)
                        k.wait("dve", s_gp, d3)
                        d4 = k.op("dve", lambda e, ti=ti, fc=fc, cs=cs: e.scalar_tensor_tensor(out=hidT[:, fc, cs], in0=u1[ti][:, :], scalar=1.0, in1=gp[ti][:, :], op0=ALU.add, op1=ALU.mult), sig=s_hid)
                        k.wait("dve", s_hid, d4)
                        r_et.release(ti, "dve", lambda e: e.tensor_copy(out=scr[0:1, 2:3], in_=scr[0:1, 3:4]))
                r_wg.release(gi, "pe", c["dummy"]["pe"])
                r_wu.release(ui, "pe", c["dummy"]["pe"])
            gud_val[ei] = k.op("pe", c["dummy"]["pe"], sig=s_gud)
            hid_ready = s_hid.n
            k.wait("pe", s_hid, hid_ready)
            k.wait("pe", r_gu.rel)
            for dg in range(4):
                wi = r_wd.next("pool")
                wv = k.dma("pool", wdb[wi][:], w_d[ei, :, dg * 512:(dg + 1) * 512].rearrange("(kk p) n -> p kk n", p=128), s_wd[wi])
                k.wait("pe", s_wd[wi], wv)
                for j in range(NJ):
                    with Guard(k, c, ("pe", "act", "sync"), TS * j):
                        for hh in range(2):
                            bank = r_dn.next("pe")
                            ps = psb[bank]
                            s0 = j * TS + hh * 128
                            for fc in range(KD):
                                k.op("pe", lambda e, fc=fc, ps=ps, wi=wi, s0=s0: e.matmul(ps[:, :], lhsT=hidT[:, fc, s0:s0 + 128], rhs=wdb[wi][:, fc, :], start=(fc == 0), stop=(fc == KD - 1)),
                                     sig=(s_dn if fc == KD - 1 else None))
                            pv = s_dn.n
                            oi = r_ob.next("act")
                            k.wait("act", s_dn, pv)
                            av = r_dn.release(bank, "act", lambda e, ps=ps, oi=oi: e.activation(out=ob[oi][:, :], in_=ps[:, :], func=AF.Copy))
                            k.wait("sync", r_dn.rel, av)
                            row = ei * CSB + s0
                            r_ob.release_dma(oi, "sync", osd[row:row + 128, dg * 512:(dg + 1) * 512], ob[oi][:, :])
                r_wd.release(wi, "pe", c["dummy"]["pe"])
            dnd_val[ei] = k.op("pe", c["dummy"]["pe"], sig=s_dnd)
        for eng in ENGS:
            r_ob.wait_all(eng)
            k.wait(eng, s_dnd)
        k.end_stage()


SWIGLU_LIMIT = 7.0
SWIGLU_ALPHA = 1.702


def stage_combine(c):
    print("sbuf before combine", c["nc"].sbuf_bytes_remaining)
    k, nc, psb = c["k"], c["nc"], c["psb"]
    x1d, osd, out, b_d = c["x1d"], c["osd"], c["out"], c["b_d"]
    w4, slot4i, WdT, gate2 = c["w4"], c["slot4i"], c["WdT"], c["gate2"]
    with contextlib.ExitStack() as st:
        sbt = lambda n, s, d: st.enter_context(nc.sbuf_tensor(n, list(s), d))
        x1 = [sbt(f"f_x{i}", [128, D], F32) for i in range(2)]
        gb = [[sbt(f"f_g{b}{q}", [128, D], F32) for q in range(4)] for b in range(2)]
        acc = [sbt(f"f_a{i}", [128, D], F32) for i in range(2)]
        bd = sbt("f_bd", [NE, D], F32)
        s_i = k.sem("f_i")
        k.dma("sync", bd[:], b_d, s_i)
        k.wait("pe", s_i)
        s_x = [k.sem("f_x0"), k.sem("f_x1")]
        s_g = [k.sem("f_g0"), k.sem("f_g1")]
        r_g = Ring(k, "f_rg", 2)
        r_x = Ring(k, "f_rx", 2)
        r_ps = Ring(k, "f_rps", 2)
        r_a = Ring(k, "f_ra", 2)
        s_pe = k.sem("f_pe")
        s_d = k.sem("f_d")
        for i in range(NTO):
            xb = r_x.next("sync")
            xvv = k.dma("sync", x1[xb][:], x1d[i * 128:(i + 1) * 128, :], s_x[xb])
            gbi = r_g.next("pool")
            for kq in range(4):
                k.op("pool", lambda e, gbi=gbi, kq=kq, i=i: e.indirect_dma_start(
                    out=gb[gbi][kq][:, :], out_offset=None, in_=osd,
                    in_offset=bass.IndirectOffsetOnAxis(ap=slot4i[:, i, kq:kq + 1], axis=0)), sig=s_g[gbi], inc=16)
            gv = s_g[gbi].n
            pset = r_ps.next("pe")
            for dg in range(4):
                k.op("pe", lambda e, pset=pset, dg=dg, i=i: e.matmul(psb[4 * pset + dg][:, :], lhsT=WdT[:, i, :], rhs=bd[:, dg * 512:(dg + 1) * 512], start=True, stop=True),
                     sig=(s_pe if dg == 3 else None))
            pv = s_pe.n
            ai = r_a.next("dve")
            k.wait("dve", s_g[gbi], gv)
            fns = [lambda e, ai=ai, gbi=gbi, i=i: e.tensor_scalar(out=acc[ai][:, :], in0=gb[gbi][0][:, :], scalar1=w4[:, i, 0:1], scalar2=None, op0=ALU.mult)]
            for kq in range(1, 4):
                fns.append(lambda e, ai=ai, gbi=gbi, i=i, kq=kq: e.scalar_tensor_tensor(out=acc[ai][:, :], in0=gb[gbi][kq][:, :], scalar=w4[:, i, kq:kq + 1], in1=acc[ai][:, :], op0=ALU.mult, op1=ALU.add))
            d1 = chain(k, "dve", s_d, fns)
            k.wait("dve", s_d, d1)
            r_g.release(gbi, "dve", lambda e, ai=ai: e.tensor_copy(out=acc[ai][0:1, 0:1], in_=acc[ai][0:1, 0:1]))
            k.wait("dve", s_pe, pv)
            k.wait("dve", r_g.rel)
            fns = [lambda e, ai=ai, pset=pset, dg=dg: e.tensor_tensor(out=acc[ai][:, dg * 512:(dg + 1) * 512], in0=acc[ai][:, dg * 512:(dg + 1) * 512], in1=psb[4 * pset + dg][:, :], op=ALU.add) for dg in range(4)]
            d2 = chain(k, "dve", s_d, fns)
            k.wait("dve", s_d, d2)
            r_ps.release(pset, "dve", lambda e, ai=ai: e.tensor_copy(out=acc[ai][0:1, 0:1], in_=acc[ai][0:1, 0:1]))
            k.wait("dve", r_ps.rel)
            k.wait("dve", s_x[xb], xvv)
            d3 = chain(k, "dve", s_d, [
                lambda e, ai=ai: e.tensor_tensor(out=acc[ai][:, :], in0=acc[ai][:, :], in1=gate2[:, :], op=ALU.mult),
            ])
            k.wait("dve", s_d, d3)
            d4 = r_x.release(xb, "dve", lambda e, ai=ai, xb=xb: e.tensor_tensor(out=acc[ai][:, :], in0=acc[ai][:, :], in1=x1[xb][:, :], op=ALU.add))
            k.wait("sync", r_x.rel, d4)
            r_a.release_dma(ai, "sync", out[i * 128:(i + 1) * 128, :], acc[ai][:, :])
        for eng in ENGS:
            r_a.wait_all(eng)
        k.end_stage()
```
